# Optimizing a Trainium2 kernel written in Bass

```python
import jax, jax.numpy as jnp
from jax import lax
import numpy as np

D_MODEL = 2048
BATCH = 4
SEQ = 8192
DEPTH = 1
DEC_BATCH = 2
DEC_SEQ = 16384
PAST_LEN = 128

HEAD_DIM = 128
A_Q_HEADS = 8
A_KV_HEADS = 2
A_GROUP = A_Q_HEADS // A_KV_HEADS
B_PAIRS = ((128, 1), (512, 4), (2048, 16))
B_N_GROUPS = len(B_PAIRS)
B_HEADS_PER_GROUP = 4
GRID_W = 64
AXIAL_THETA = 10000.0
ROPE_THETA = 500000.0
ROPE_DIM = HEAD_DIM // 4
Q_BLOCK = 128
NORM_EPS = 1e-6
NEG_INF = -1e30
ATTN_SCALE = HEAD_DIM ** -0.5

A_Q_W = A_Q_HEADS * HEAD_DIM
A_KV_W = A_KV_HEADS * HEAD_DIM
B_W = B_N_GROUPS * B_HEADS_PER_GROUP * HEAD_DIM
B_OUT_W = B_HEADS_PER_GROUP * HEAD_DIM
IN_WIDTH = A_Q_W + 2 * A_KV_W + 3 * B_W + 2 * D_MODEL
SPLITS = tuple(int(v) for v in np.cumsum([A_Q_W, A_KV_W, A_KV_W, B_W, B_W, B_W, D_MODEL]))

N_EXPERT_GROUPS = 4
EXPERTS_PER_GROUP = 8
N_EXPERTS = N_EXPERT_GROUPS * EXPERTS_PER_GROUP
TOP_K = 2
D_EXPERT = 512
ROUTE_BLOCK = 128

kernel_name = 'hybrid_axial_gqa_dilated_hmoe_encoder'


def rmsnorm(x, g):
    xf = x.astype(jnp.float32)
    y = xf * lax.rsqrt(jnp.mean(xf * xf, axis=-1, keepdims=True) + NORM_EPS)
    return (y * g.astype(jnp.float32)).astype(x.dtype)


def rope(x, pos, theta):
    half = x.shape[-1] // 2
    inv = jnp.power(jnp.float32(theta), -jnp.arange(half, dtype=jnp.float32) / half)
    ang = pos.astype(jnp.float32)[:, None] * inv[None, :]
    cos = jnp.cos(ang)[None, :, None, :]
    sin = jnp.sin(ang)[None, :, None, :]
    xf = x.astype(jnp.float32)
    x1, x2 = xf[..., :half], xf[..., half:]
    return jnp.concatenate([x1 * cos - x2 * sin, x2 * cos + x1 * sin], axis=-1).astype(x.dtype)


def axial_rope(x, seq_len):
    rows = seq_len // GRID_W
    r_idx = jnp.repeat(jnp.arange(rows), GRID_W)
    c_idx = jnp.tile(jnp.arange(GRID_W), rows)
    half = x.shape[-1] // 2
    return jnp.concatenate([rope(x[..., :half], r_idx, AXIAL_THETA),
                            rope(x[..., half:], c_idx, AXIAL_THETA)], axis=-1)


def partial_rope(x, seq_len):
    pos = jnp.arange(seq_len)
    return jnp.concatenate([rope(x[..., :ROPE_DIM], pos, ROPE_THETA), x[..., ROPE_DIM:]], axis=-1)


def gqa_attention(q, k, v):
    bsz, s = q.shape[0], q.shape[1]
    nb = s // Q_BLOCK
    qb = q.reshape(bsz, nb, Q_BLOCK, A_KV_HEADS, A_GROUP, HEAD_DIM).transpose(1, 0, 2, 3, 4, 5)

    def block(qi):
        sc = jnp.einsum('bqgrd,bkgd->bgrqk', qi, k, preferred_element_type=jnp.float32) * ATTN_SCALE
        p = jax.nn.softmax(sc, axis=-1)
        return jnp.einsum('bgrqk,bkgd->bqgrd', p.astype(v.dtype), v)

    o = lax.map(block, qb)
    return o.transpose(1, 0, 2, 3, 4, 5).reshape(bsz, s, A_Q_W)


def banded_dilated(q, k, v, window, dilation):
    bsz, s, h, d = q.shape
    L = s // dilation
    n = window // (2 * dilation)
    nb = -(-L // n)
    lp = nb * n
    N = bsz * dilation

    def fold(t):
        return t.reshape(bsz, L, dilation, h, d).transpose(0, 2, 1, 3, 4).reshape(N, L, h, d)

    qf, kf, vf = fold(q), fold(k), fold(v)
    qp = jnp.pad(qf, ((0, 0), (0, lp - L), (0, 0), (0, 0))).reshape(N, nb, n, h, d)

    def windows(t):
        tp = jnp.pad(t, ((0, 0), (n, lp - L + n), (0, 0), (0, 0))).reshape(N, nb + 2, n, h, d)
        return jnp.concatenate([tp[:, :-2], tp[:, 1:-1], tp[:, 2:]], axis=2)

    kw, vw = windows(kf), windows(vf)
    sc = jnp.einsum('nbqhd,nbkhd->nbhqk', qp, kw, preferred_element_type=jnp.float32) * ATTN_SCALE
    i = jnp.arange(n)[:, None]
    j = jnp.arange(3 * n)[None, :]
    band = jnp.abs(j - n - i) <= n
    key_u = (jnp.arange(nb)[:, None] - 1) * n + jnp.arange(3 * n)[None, :]
    kvalid = (key_u >= 0) & (key_u < L)
    mask = band[None, :, :] & kvalid[:, None, :]
    sc = jnp.where(mask[None, :, None, :, :], sc, NEG_INF)
    m = jnp.max(sc, axis=-1, keepdims=True)
    p = jnp.exp(sc - m)
    den = jnp.sum(p, axis=-1)
    o = jnp.einsum('nbhqk,nbkhd->nbqhd', p.astype(v.dtype), vw).astype(jnp.float32)
    den_q = den.transpose(0, 1, 3, 2)
    o = o / den_q[..., None]
    lse = (m[..., 0] + jnp.log(den)).transpose(0, 1, 3, 2)

    def unfold(t):
        rest = t.shape[3:]
        t = t.reshape((N, lp) + rest)[:, :L]
        t = jnp.swapaxes(t.reshape((bsz, dilation, L) + rest), 1, 2)
        return t.reshape((bsz, s) + rest)

    return unfold(o), unfold(lse)


def dilated_mixture(qb, kb, vb):
    bsz, s = qb.shape[0], qb.shape[1]
    outs, lses = [], []
    for g, (w, r) in enumerate(B_PAIRS):
        o, l = banded_dilated(qb[:, :, g], kb[:, :, g], vb[:, :, g], w, r)
        outs.append(o)
        lses.append(l)
    alpha = jax.nn.softmax(jnp.stack(lses, axis=0), axis=0)
    o = jnp.sum(alpha[..., None] * jnp.stack(outs, axis=0), axis=0)
    return o.reshape(bsz, s, B_OUT_W).astype(qb.dtype)


def hier_moe(h, w_rg, b_rg, w_re, b_re, w1, w3, w2):
    T = h.shape[0]
    pg = jax.nn.softmax((h @ w_rg).astype(jnp.float32) + b_rg, axis=-1)
    g_sel = jnp.argmax(pg, axis=-1)
    g_w = jnp.take_along_axis(pg, g_sel[:, None], axis=-1)[:, 0]
    le = ((h @ w_re).astype(jnp.float32) + b_re).reshape(T, N_EXPERT_GROUPS, EXPERTS_PER_GROUP)
    le_sel = jnp.take_along_axis(le, g_sel[:, None, None], axis=1)[:, 0]
    top_p, top_i = lax.top_k(jax.nn.softmax(le_sel, axis=-1), TOP_K)
    top_p = top_p / jnp.sum(top_p, axis=-1, keepdims=True)
    wts = (g_w[:, None] * top_p).reshape(-1)
    e_id = (g_sel[:, None] * EXPERTS_PER_GROUP + top_i).reshape(-1).astype(jnp.int32)
    tok = jnp.repeat(jnp.arange(T, dtype=jnp.int32), TOP_K)
    A = T * TOP_K
    order = jnp.argsort(e_id)
    e_s, tok_s, w_s = e_id[order], tok[order], wts[order]
    counts = jnp.bincount(e_id, length=N_EXPERTS).astype(jnp.int32)
    starts = jnp.cumsum(counts) - counts
    pcounts = (counts + ROUTE_BLOCK - 1) // ROUTE_BLOCK * ROUTE_BLOCK
    pends = jnp.cumsum(pcounts)
    pstarts = pends - pcounts
    dest = pstarts[e_s] + (jnp.arange(A, dtype=jnp.int32) - starts[e_s])
    P = A + N_EXPERTS * ROUTE_BLOCK
    nblk = P // ROUTE_BLOCK
    row_tok = jnp.full((P,), T, jnp.int32).at[dest].set(tok_s)
    row_w = jnp.zeros((P,), jnp.float32).at[dest].set(w_s)
    blk_e = jnp.minimum(jnp.searchsorted(pends, jnp.arange(nblk, dtype=jnp.int32) * ROUTE_BLOCK,
                                         side='right'), N_EXPERTS - 1)
    hp = jnp.concatenate([h, jnp.zeros((1, h.shape[1]), h.dtype)], axis=0)
    xin = hp[row_tok].reshape(nblk, ROUTE_BLOCK, h.shape[1])

    def expert_block(args):
        xb, e = args
        return (jax.nn.silu(xb @ w1[e]) * (xb @ w3[e])) @ w2[e]

    out = lax.map(expert_block, (xin, blk_e)).reshape(P, h.shape[1])
    out = out * row_w[:, None].astype(out.dtype)
    return jnp.zeros((T + 1, h.shape[1]), out.dtype).at[row_tok].add(out)[:T]


def layer(x, g_mix, w_in, g_qa, g_ka, g_qb, g_kb, w_oa, w_ob, w_out,
          g_ffn, w_rg, b_rg, w_re, b_re, w1, w3, w2):
    bsz, s, _ = x.shape
    h = rmsnorm(x, g_mix)
    proj = h @ w_in
    qa, ka, va, qb, kb, vb, ga, gb = jnp.split(proj, SPLITS, axis=-1)
    qa = axial_rope(rmsnorm(qa.reshape(bsz, s, A_Q_HEADS, HEAD_DIM), g_qa), s)
    ka = axial_rope(rmsnorm(ka.reshape(bsz, s, A_KV_HEADS, HEAD_DIM), g_ka), s)
    va = va.reshape(bsz, s, A_KV_HEADS, HEAD_DIM)
    ya = gqa_attention(qa, ka, va) @ w_oa
    nbh = B_N_GROUPS * B_HEADS_PER_GROUP
    bshape = (bsz, s, B_N_GROUPS, B_HEADS_PER_GROUP, HEAD_DIM)
    qb = partial_rope(rmsnorm(qb.reshape(bsz, s, nbh, HEAD_DIM), g_qb), s).reshape(bshape)
    kb = partial_rope(rmsnorm(kb.reshape(bsz, s, nbh, HEAD_DIM), g_kb), s).reshape(bshape)
    vb = vb.reshape(bshape)
    yb = dilated_mixture(qb, kb, vb) @ w_ob
    mixed = jax.nn.sigmoid(ga) * ya + jax.nn.sigmoid(gb) * yb
    x = x + mixed @ w_out
    h2 = rmsnorm(x, g_ffn).reshape(bsz * s, D_MODEL)
    return x + hier_moe(h2, w_rg, b_rg, w_re, b_re, w1, w3, w2).reshape(bsz, s, D_MODEL)


def setup_inputs(seed: int = 0) -> dict:
    key = jax.random.key(seed)
    ks = jax.random.split(key, 19)
    f32 = jnp.float32

    def nrm(k, shape, scale):
        return jax.random.normal(k, shape, f32) * scale

    def gain(k, shape):
        return 1.0 + 0.02 * jax.random.normal(k, shape, f32)

    return {
        'x_prompt': nrm(ks[0], (BATCH, SEQ, D_MODEL), 1.0),
        'x_sample': nrm(ks[1], (DEC_BATCH, DEC_SEQ, D_MODEL), 1.0),
        'g_mix': gain(ks[2], (DEPTH, D_MODEL)),
        'w_in': nrm(ks[3], (DEPTH, D_MODEL, IN_WIDTH), D_MODEL ** -0.5),
        'g_qa': gain(ks[4], (DEPTH, HEAD_DIM)),
        'g_ka': gain(ks[5], (DEPTH, HEAD_DIM)),
        'g_qb': gain(ks[6], (DEPTH, HEAD_DIM)),
        'g_kb': gain(ks[7], (DEPTH, HEAD_DIM)),
        'w_oa': nrm(ks[8], (DEPTH, A_Q_W, D_MODEL), A_Q_W ** -0.5),
        'w_ob': nrm(ks[9], (DEPTH, B_OUT_W, D_MODEL), B_OUT_W ** -0.5),
        'w_out': nrm(ks[10], (DEPTH, D_MODEL, D_MODEL), D_MODEL ** -0.5),
        'g_ffn': gain(ks[11], (DEPTH, D_MODEL)),
        'w_rg': nrm(ks[12], (DEPTH, D_MODEL, N_EXPERT_GROUPS), D_MODEL ** -0.5),
        'b_rg': nrm(ks[13], (DEPTH, N_EXPERT_GROUPS), 0.01),
        'w_re': nrm(ks[14], (DEPTH, D_MODEL, N_EXPERTS), D_MODEL ** -0.5),
        'b_re': nrm(ks[15], (DEPTH, N_EXPERTS), 0.01),
        'w1': nrm(ks[16], (DEPTH, N_EXPERTS, D_MODEL, D_EXPERT), D_MODEL ** -0.5),
        'w3': nrm(ks[17], (DEPTH, N_EXPERTS, D_MODEL, D_EXPERT), D_MODEL ** -0.5),
        'w2': nrm(ks[18], (DEPTH, N_EXPERTS, D_EXPERT, D_MODEL), D_EXPERT ** -0.5),
    }


def reference(x_prompt, x_sample, g_mix, w_in, g_qa, g_ka, g_qb, g_kb, w_oa, w_ob, w_out,
              g_ffn, w_rg, b_rg, w_re, b_re, w1, w3, w2):
    y_prompt = x_prompt
    y_sample = x_sample
    for l in range(DEPTH):
        args = (g_mix[l], w_in[l], g_qa[l], g_ka[l], g_qb[l], g_kb[l], w_oa[l], w_ob[l], w_out[l],
                g_ffn[l], w_rg[l], b_rg[l], w_re[l], b_re[l], w1[l], w3[l], w2[l])
        y_prompt = layer(y_prompt, *args)
        y_sample = layer(y_sample, *args)
    return (y_prompt, y_sample)
```

```python
import math
import numpy as np
from contextlib import ExitStack
import concourse.bass as bass
import concourse.mybir as mybir
from concourse.bass_utils import run_bass_kernel_spmd

F32 = mybir.dt.float32
BF16 = mybir.dt.bfloat16
I32 = mybir.dt.int32
ALU = mybir.AluOpType
AF = mybir.ActivationFunctionType
AX = mybir.AxisListType

D = 2048
KC = 16
HD = 128
INW = 10240
C_QA, C_KA, C_VA, C_QB, C_KB, C_VB, C_GA, C_GB = 0, 1024, 1280, 1536, 3072, 4608, 6144, 8192
HALO = 1024
NE = 32
DE = 512
EPS = 1e-6
SCALE = HD ** -0.5
B_DIL = (1, 4, 16)
N_HW_SEMS = 24
N_SW_SEMS = 32
TB = 4


class Tracker:
    def __init__(self, nc, es):
        self.nc = nc
        self.eng = {'pe': nc.tensor, 'act': nc.scalar, 'dve': nc.vector, 'pool': nc.gpsimd, 'sp': nc.sync}
        self.sem = {k: es.enter_context(nc.semaphore('s_' + k)) for k in self.eng}
        self.dsem = [es.enter_context(nc.semaphore('d_%d' % i)) for i in range(N_HW_SEMS)]
        self.swsem = [es.enter_context(nc.semaphore('w_%d' % i)) for i in range(N_SW_SEMS)]
        self.swcnt = [0] * N_SW_SEMS
        self.swlast = [None] * N_SW_SEMS
        self.reset_state()

    def reset_state(self):
        self.cnt = {k: 0 for k in self.eng}
        self.known = {k: {} for k in self.eng}
        self.dcnt = [0] * N_HW_SEMS
        self.dpending = [None] * N_HW_SEMS
        self.dnext = 0
        self.swnext = 0
        self.swpending = {}
        self.lastw = {}
        self.reads = {}

    def _semof(self, key):
        if isinstance(key, str):
            return self.sem[key]
        if isinstance(key, tuple):
            return self.swsem[key[1]]
        return self.dsem[key]

    def _wait(self, e, ev):
        if ev is None:
            return
        key, val = ev
        if self.known[e].get(key, 0) >= val:
            return
        if key == 'pe' and e == 'pe':
            return
        self.eng[e].wait_ge(self._semof(key), val)
        self.known[e][key] = val
        if isinstance(key, int):
            p = self.dpending[key]
            if p is not None and p[1] <= val:
                self.dpending[key] = None
        elif isinstance(key, tuple):
            p = self.swpending.get(key)
            if p is not None and p[1] <= val:
                self.swpending.pop(key)

    def _deps(self, e, reads, writes):
        for r in reads:
            self._wait(e, self.lastw.get(r))
        for w in writes:
            self._wait(e, self.lastw.get(w))
            for ev in self.reads.get(w, ()):
                self._wait(e, ev)

    def _record(self, ev, reads, writes):
        for r in reads:
            lst = self.reads.setdefault(r, [])
            lst.append(ev)
            if len(lst) > 48:
                best = {}
                for k, v in lst:
                    best[k] = max(best.get(k, 0), v)
                self.reads[r] = list(best.items())
        for w in writes:
            self.lastw[w] = ev
            self.reads[w] = []

    cut = None
    nops = 0
    sw_clear = False

    def op(self, e, fn, reads=(), writes=()):
        Tracker.nops += 1
        if Tracker.cut is not None and Tracker.nops > Tracker.cut:
            return None
        self._deps(e, reads, writes)
        ins = fn(self.eng[e])
        self.cnt[e] += 1
        ins.then_inc(self.sem[e], 1)
        ev = (e, self.cnt[e])
        self._record(ev, reads, writes)
        return ev

    def dma(self, e, fn, reads=(), writes=()):
        Tracker.nops += 1
        if Tracker.cut is not None and Tracker.nops > Tracker.cut:
            return None
        if e == 'pool':
            if Tracker.sw_clear and self.swnext >= N_SW_SEMS:
                self.sync_all()
            self._deps(e, reads, writes)
            si = self.swnext % N_SW_SEMS
            self.swnext += 1
            if self.swlast[si] is not None:
                self._wait('pool', self.swlast[si])
            ins = fn(self.eng[e])
            self.swcnt[si] += 16
            ins.then_inc(self.swsem[si], 16)
            ev = (('w', si), self.swcnt[si])
            self.swlast[si] = ev
            self.swpending[('w', si)] = ev
        else:
            self._deps(e, reads, writes)
            i = self.dnext
            self.dnext = (self.dnext + 1) % N_HW_SEMS
            p = self.dpending[i]
            if p is not None:
                self._wait(e, (i, p[1]))
                self.dpending[i] = None
            ins = fn(self.eng[e])
            self.dcnt[i] += 16
            ins.then_inc(self.dsem[i], 16)
            ev = (i, self.dcnt[i])
            self.dpending[i] = (e, self.dcnt[i])
        self._record(ev, reads, writes)
        return ev

    def drain(self):
        for key, ev in list(self.swpending.items()):
            self._wait('pool', ev)
        for i in range(N_HW_SEMS):
            p = self.dpending[i]
            if p is not None:
                self._wait(p[0], (i, p[1]))
                self.dpending[i] = None

    def sync_all(self):
        self.drain()
        nc = self.nc
        nc.all_engine_barrier()
        for k in self.eng:
            self.eng[k].sem_clear(self.sem[k])
        for i in range(N_HW_SEMS):
            if self.dcnt[i]:
                nc.sync.sem_clear(self.dsem[i])
        if Tracker.sw_clear:
            for i in range(min(self.swnext, N_SW_SEMS)):
                nc.gpsimd.sem_clear(self.swsem[i])
            self.swcnt = [0] * N_SW_SEMS
        self.swlast = [None] * N_SW_SEMS
        nc.all_engine_barrier()
        self.reset_state()

    def maybe_sync(self, limit=24000):
        if max(self.cnt.values()) > limit or max(self.dcnt) > limit:
            self.sync_all()


def build(OWN, CAP, stages=None, dbg=()):
    SEQS = (2 * OWN, 4 * OWN)
    REST = (SEQS[0] - OWN, SEQS[1] - OWN)
    EXT = OWN + 2 * HALO
    NOWN = 2 * OWN
    NT_OWN = NOWN // 128
    XROWS = NE * CAP
    on = (lambda s: True) if stages is None else (lambda s: s in stages)

    nc = bass.Bass("TRN2", target_bir_lowering=False)

    in_names = []

    def din(name, shape, dt=F32):
        in_names.append(name)
        return nc.dram_tensor(name, list(shape), dt, kind="ExternalInput").ap()

    def dscr(name, shape, dt):
        kind = "ExternalOutput" if name in dbg else "Internal"
        return nc.dram_tensor(name, list(shape), dt, kind=kind).ap()

    xo = din("xo", [NOWN, D]); xr = din("xr", [REST[0] + REST[1], D]); xh = din("xh", [4 * HALO, D])
    tA_o = din("tA_o", [NOWN, 128]); tB_o = din("tB_o", [NOWN, 32])
    tA_r = din("tA_r", [REST[0] + REST[1], 128]); tB_h = din("tB_h", [4 * HALO, 32])
    vh = din("vh", [4 * HALO // TB, TB])
    g_mix = din("g_mix", [1, D]); g_ffn = din("g_ffn", [1, D])
    g_qa = din("g_qa", [1, HD]); g_ka = din("g_ka", [1, HD]); g_qb = din("g_qb", [1, HD]); g_kb = din("g_kb", [1, HD])
    w_in = din("w_in", [D, INW]); w_oa = din("w_oa", [1024, D]); w_ob = din("w_ob", [512, D]); w_out = din("w_out", [D, D])
    w_r = din("w_r", [D, 36]); b_r = din("b_r", [1, 36])
    if on('experts'):
        w1 = din("w1", [NE, D, DE]); w3 = din("w3", [NE, D, DE]); w2 = din("w2", [NE, DE, D])
    y = nc.dram_tensor("y", [NOWN, D], F32, kind="ExternalOutput").ap()

    hT_own = dscr("hT_own", [KC, 128, NOWN], BF16)
    qaT = dscr("qaT", [8, 128, NOWN], BF16)
    kaT = [dscr("kaT%d" % c, [2, 128, SEQS[c]], BF16) for c in range(2)]
    va = [dscr("va%d" % c, [SEQS[c], 256], BF16) for c in range(2)]
    qbT = dscr("qbT", [12, 128, NOWN], BF16)
    kbT = [dscr("kbT%d" % c, [12, 128, EXT], BF16) for c in range(2)]
    vbx = [dscr("vbx%d" % c, [EXT, 12 * 129], BF16) for c in range(2)]
    sg = dscr("sg", [NOWN, 4096], BF16)
    aoT = dscr("aoT", [8, 128, NOWN], BF16)
    bnum = dscr("bnum", [3, NOWN, 4 * 129], F32)
    x_mid = dscr("x_mid", [NOWN, D], F32)
    h2 = dscr("h2", [NOWN, D], BF16)
    xin = dscr("xin", [XROWS, D], BF16)
    eout = dscr("eout", [XROWS, D], BF16)
    dest_d = dscr("dest_d", [NT_OWN * 128, 2], I32)
    wts_d = dscr("wts_d", [NT_OWN * 128, 2], F32)

    with ExitStack() as es:
        T = Tracker(nc, es)

        def sb(es_, name, shape, dt):
            return es_.enter_context(nc.sbuf_tensor(name, list(shape), dt))

        def ps(es_, name, shape, dt):
            return es_.enter_context(nc.psum_tensor(name, list(shape), dt))

        ident_b = sb(es, "ident_b", [128, 128], BF16)
        ident_f = sb(es, "ident_f", [128, 128], F32)
        ones_b = sb(es, "ones_b", [128, 128], BF16)
        ones_f = sb(es, "ones_f", [128, 128], F32)
        gh = sb(es, "gh", [128, 4, HD], F32)
        negC = sb(es, "negC", [128, 2], F32)
        gmx = sb(es, "gmx", [128, 4], F32)
        gh2 = sb(es, "gh2", [128, 4, HD], F32)
        epsb = sb(es, "epsb", [128, 1], F32)

        for t_, nm in ((ident_b, 'ident_b'), (ident_f, 'ident_f')):
            T.op('pool', lambda g, t_=t_: g.memset(t_[:], 1.0), writes=[nm])
            T.op('pool', lambda g, t_=t_: g.affine_select(out=t_[:], in_=t_[:], pattern=[[-1, 128]], compare_op=ALU.is_equal,
                                                          fill=0.0, base=0, channel_multiplier=1), reads=[nm], writes=[nm])
        T.op('pool', lambda g: g.memset(ones_b[:], 1.0), writes=['ones_b'])
        T.op('pool', lambda g: g.memset(ones_f[:], 1.0), writes=['ones_f'])
        T.op('pool', lambda g: g.memset(epsb[:], EPS), writes=['epsb'])
        for k_, gsrc in enumerate((g_qa, g_ka, g_qb, g_kb)):
            T.dma('sp', lambda e, k_=k_, gsrc=gsrc: e.dma_start(out=gh[:, k_, :], in_=gsrc.to_broadcast([128, HD])), writes=['gh'])
        T.op('dve', lambda e: e.tensor_scalar(out=gh2[:], in0=gh[:], scalar1=-1.0, scalar2=None, op0=ALU.mult), reads=['gh'], writes=['gh2'])
        T.op('dve', lambda e: e.tensor_tensor(out=gh2[:], in0=gh2[:], in1=gh[:], op=ALU.max), reads=['gh', 'gh2'], writes=['gh2'])
        T.op('dve', lambda e: e.tensor_reduce(out=gmx[:], in_=gh2[:], axis=AX.X, op=ALU.max), reads=['gh2'], writes=['gmx'])
        T.op('dve', lambda e: e.tensor_tensor(out=negC[:, 0:1], in0=gmx[:, 0:1], in1=gmx[:, 1:2], op=ALU.mult), reads=['gmx'], writes=['negC'])
        T.op('dve', lambda e: e.tensor_tensor(out=negC[:, 1:2], in0=gmx[:, 2:3], in1=gmx[:, 3:4], op=ALU.mult), reads=['gmx', 'negC'], writes=['negC'])
        T.op('dve', lambda e: e.tensor_scalar(out=negC[:], in0=negC[:], scalar1=-math.sqrt(HD), scalar2=None, op0=ALU.mult), reads=['negC'], writes=['negC'])
        T.sync_all()

        def proj_pass(pname, ntok, wsegs, from_x, x_src, hT_src, hT_dst, make_groups):
            WC = sum(w for _, w in wsegs)
            with ExitStack() as s2:
                wt = sb(s2, pname + "_wt", [128, KC, WC], BF16)
                hst = sb(s2, pname + "_hst", [128, KC, TB * 128], BF16)
                xt = [sb(s2, pname + "_xt%d" % k, [128, D], F32) for k in range(2)] if from_x else None
                hb = [sb(s2, pname + "_hb%d" % k, [128, D], BF16) for k in range(2)] if from_x else None
                junk = sb(s2, pname + "_junk", [128, D], BF16) if from_x else None
                gmix_b = sb(s2, pname + "_gmix", [128, D], F32) if from_x else None
                if from_x:
                    T.dma('sp', lambda e: e.dma_start(out=gmix_b[:], in_=g_mix.to_broadcast([128, D])), writes=['gmix_b'])
                ss = sb(s2, pname + "_ss", [128, 2], F32) if from_x else None
                pt = ps(s2, pname + "_pt", [128, KC, 128], BF16) if from_x else None
                pj = [ps(s2, pname + "_pj%d" % k, [128, 512], F32) for k in range(3)]
                ctx = dict(s2=s2, pname=pname)
                groups, flush = make_groups(ctx)
                off = 0
                for (c0, w) in wsegs:
                    for kc in range(0, KC, 4):
                        T.dma('pool', lambda g, c0=c0, w=w, off=off, kc=kc: g.dma_start(
                            out=wt[:, kc:kc + 4, off:off + w],
                            in_=w_in[kc * 128:(kc + 4) * 128, c0:c0 + w].rearrange("(c p) n -> p c n", p=128)), writes=['wt'])
                    off += w
                T.sync_all()
                nsup = ntok // (TB * 128)

                def xpath(gt_):
                    b = gt_ % 2
                    T.dma('sp', lambda e: e.dma_start(out=xt[b][:], in_=x_src[gt_ * 128:(gt_ + 1) * 128, :]), writes=['xt%d' % b])
                    T.op('pool', lambda e: e.memset(ss[:, b:b + 1], 0.0), writes=['ss%d' % b])
                    T.op('act', lambda e: e.activation(out=junk[:], in_=xt[b][:], func=AF.Square, accum_out=ss[:, b:b + 1]),
                         reads=['xt%d' % b, 'ss%d' % b], writes=['junk', 'ss%d' % b])
                    T.op('act', lambda e: e.activation(out=ss[:, b:b + 1], in_=ss[:, b:b + 1], func=AF.Sqrt, bias=epsb[:], scale=1.0 / D),
                         reads=['ss%d' % b, 'epsb'], writes=['ss%d' % b])
                    T.op('dve', lambda e: e.reciprocal(out=ss[:, b:b + 1], in_=ss[:, b:b + 1]), reads=['ss%d' % b], writes=['ss%d' % b])
                    T.op('dve', lambda e: e.scalar_tensor_tensor(out=hb[b][:], in0=xt[b][:], scalar=ss[:, b:b + 1], in1=gmix_b[:],
                                                                  op0=ALU.mult, op1=ALU.mult),
                         reads=['xt%d' % b, 'ss%d' % b, 'gmix_b'], writes=['hb%d' % b])

                if from_x:
                    xpath(0)
                pending = None
                gi = 0
                for i in range(nsup):
                    if not from_x:
                        T.dma('sp', lambda e: e.dma_start(out=hst[:], in_=hT_src[:, :, bass.ts(i, TB * 128)].rearrange("c p t -> p c t")),
                              writes=['hst'])
                    for t in range(TB):
                        tsl = slice(t * 128, (t + 1) * 128)
                        if from_x:
                            gt_ = i * TB + t
                            b = gt_ % 2
                            for c in range(KC):
                                T.op('pe', lambda e, b=b, c=c: e.transpose(out=pt[:, c, :], in_=hb[b][:, c * 128:(c + 1) * 128], identity=ident_b[:]),
                                     reads=['hb%d' % b, 'ident_b'], writes=['pt%d' % (c // 8)])
                            T.op('act', lambda e, tsl=tsl: e.copy(out=hst[:, 0:8, tsl], in_=pt[:, 0:8, :]),
                                 reads=['pt0'], writes=['hst%d' % t])
                            T.op('dve', lambda e, tsl=tsl: e.tensor_copy(out=hst[:, 8:16, tsl], in_=pt[:, 8:16, :]),
                                 reads=['pt1'], writes=['hst%da' % t])
                            if gt_ + 1 < nsup * TB:
                                xpath(gt_ + 1)
                        hres = ['hst', 'hst%d' % t, 'hst%da' % t, 'wt']
                        for (woff, width, post) in groups:
                            pb = gi % 3
                            gi += 1
                            for c in range(KC):
                                T.op('pe', lambda e, c=c, pb=pb, woff=woff, width=width, tsl=tsl: e.matmul(
                                    pj[pb][:, 0:width], lhsT=hst[:, c, tsl], rhs=wt[:, c, woff:woff + width],
                                    start=(c == 0), stop=(c == KC - 1)), reads=hres, writes=['pj%d' % pb])
                            d_ = post(pj[pb], 'pj%d' % pb, t, i)
                            if pending is not None:
                                pending()
                            pending = d_
                    if pending is not None:
                        pending()
                        pending = None
                    if hT_dst is not None:
                        T.dma('sp', lambda e: e.dma_start(out=hT_dst[:, :, bass.ts(i, TB * 128)].rearrange("c p t -> p c t"), in_=hst[:]),
                              reads=['hst'] + ['hst%d' % t for t in range(TB)] + ['hst%da' % t for t in range(TB)])
                    flush(i)
                    T.maybe_sync()
                T.sync_all()

        def mk_qk(ctx, key, nh_total, gidx, rope, tab_src, dstT):
            s2 = ctx['s2']; pn = ctx['pname'] + key
            qst = sb(s2, pn + "_qst", [128, nh_total, TB * 128], BF16)
            sq = [sb(s2, pn + "_sq%d" % k, [128, 4, HD], F32) for k in range(2)]
            yv = [sb(s2, pn + "_y%d" % k, [128, 4, HD], F32) for k in range(2)]
            yb = [sb(s2, pn + "_yb%d" % k, [128, 4, HD], BF16) for k in range(2)]
            tt = [sb(s2, pn + "_tt%d" % k, [128, 4, 4, 64], F32) for k in range(2)]
            st4 = [sb(s2, pn + "_st%d" % k, [128, 4], F32) for k in range(2)]
            tw = 128 if rope == 'axial' else 32
            tab = [sb(s2, pn + "_tab%d" % k, [128, tw], F32) for k in range(2)]
            ptq = ps(s2, pn + "_ptq", [128, 4, 128], BF16)
            state = {'n': 0, 'tabt': {}}

            def post_factory(head0, nh):
                def post(pj_t, pjn, t, i):
                    k = state['n'] % 2
                    state['n'] += 1
                    R = lambda *a: [pn + '%s%d' % (x, k) for x in a]
                    tb_ = t % 2
                    if state['tabt'].get(tb_) != t:
                        state['tabt'][tb_] = t
                        T.dma('sp', lambda e: e.dma_start(out=tab[tb_][:], in_=tab_src[bass.ts(i, TB * 128), :][t * 128:(t + 1) * 128, :]),
                              writes=[pn + 'tab%d' % tb_])
                    tabn = pn + 'tab%d' % tb_
                    P3 = pj_t[:, 0:nh * HD].rearrange("p (h d) -> p h d", h=nh)
                    T.op('act', lambda e: e.activation(out=sq[k][:, 0:nh, :], in_=P3, func=AF.Square), reads=[pjn], writes=R('sq'))
                    T.op('dve', lambda e: e.tensor_reduce(out=st4[k][:, 0:nh], in_=sq[k][:, 0:nh, :], axis=AX.X, op=ALU.add), reads=R('sq'), writes=R('st'))
                    T.op('act', lambda e: e.activation(out=st4[k][:, 0:nh], in_=st4[k][:, 0:nh], func=AF.Sqrt, bias=epsb[:], scale=1.0 / HD),
                         reads=R('st') + ['epsb'], writes=R('st'))
                    T.op('dve', lambda e: e.reciprocal(out=st4[k][:, 0:nh], in_=st4[k][:, 0:nh]), reads=R('st'), writes=R('st'))
                    T.op('dve', lambda e: e.tensor_tensor(out=yv[k][:, 0:nh, :], in0=P3, in1=st4[k][:, 0:nh].unsqueeze(2).to_broadcast([128, nh, HD]), op=ALU.mult),
                         reads=[pjn] + R('st'), writes=R('y'))
                    T.op('pool', lambda e: e.tensor_tensor(out=yv[k][:, 0:nh, :], in0=yv[k][:, 0:nh, :], in1=gh[:, gidx, :].unsqueeze(1).to_broadcast([128, nh, HD]), op=ALU.mult),
                         reads=R('y') + ['gh'], writes=R('y'))
                    if rope == 'axial':
                        Y = yv[k][:, 0:nh, :].rearrange("p h (a b f) -> p h a b f", a=2, b=2)
                        YB = yb[k][:, 0:nh, :].rearrange("p h (a b f) -> p h a b f", a=2, b=2)
                        x1, x2 = Y[:, :, :, 0, :], Y[:, :, :, 1, :]
                        cosb = tab[tb_][:, 0:64].rearrange("p (a f) -> p a f", a=2).unsqueeze(1).to_broadcast([128, nh, 2, 32])
                        sinb = tab[tb_][:, 64:128].rearrange("p (a f) -> p a f", a=2).unsqueeze(1).to_broadcast([128, nh, 2, 32])
                        TT = [tt[k][:, 0:nh, j, :].rearrange("p h (a f) -> p h a f", a=2) for j in range(4)]
                        o1, o2 = YB[:, :, :, 0, :], YB[:, :, :, 1, :]
                    else:
                        Y = yv[k][:, 0:nh, :]
                        x1, x2 = Y[:, :, 0:16], Y[:, :, 16:32]
                        cosb = tab[tb_][:, 0:16].unsqueeze(1).to_broadcast([128, nh, 16])
                        sinb = tab[tb_][:, 16:32].unsqueeze(1).to_broadcast([128, nh, 16])
                        TT = [tt[k][:, 0:nh, j, 0:16] for j in range(4)]
                        o1, o2 = yb[k][:, 0:nh, 0:16], yb[k][:, 0:nh, 16:32]
                        T.op('act', lambda e: e.copy(out=yb[k][:, 0:nh, 32:128], in_=Y[:, :, 32:128]), reads=R('y'), writes=R('yb'))
                    T.op('pool', lambda e: e.tensor_tensor(out=TT[0], in0=x1, in1=cosb, op=ALU.mult), reads=R('y') + [tabn], writes=R('tta'))
                    T.op('dve', lambda e: e.tensor_tensor(out=TT[1], in0=x2, in1=sinb, op=ALU.mult), reads=R('y') + [tabn], writes=R('ttb'))
                    T.op('pool', lambda e: e.tensor_tensor(out=o1, in0=TT[0], in1=TT[1], op=ALU.subtract), reads=R('tta', 'ttb'), writes=R('yb'))
                    T.op('dve', lambda e: e.tensor_tensor(out=TT[2], in0=x2, in1=cosb, op=ALU.mult), reads=R('y') + [tabn], writes=R('ttc'))
                    T.op('pool', lambda e: e.tensor_tensor(out=TT[3], in0=x1, in1=sinb, op=ALU.mult), reads=R('y') + [tabn], writes=R('ttd'))
                    T.op('dve', lambda e: e.tensor_tensor(out=o2, in0=TT[2], in1=TT[3], op=ALU.add), reads=R('ttc', 'ttd'), writes=R('yb'))
                    def deferred():
                        for h in range(nh):
                            T.op('pe', lambda e, h=h: e.transpose(out=ptq[:, h, :], in_=yb[k][:, h, :], identity=ident_b[:]),
                                 reads=R('yb') + ['ident_b'], writes=[pn + 'ptq'])
                        T.op('act', lambda e: e.copy(out=qst[:, head0:head0 + nh, t * 128:(t + 1) * 128], in_=ptq[:, 0:nh, :]),
                             reads=[pn + 'ptq'], writes=[pn + 'qst'])
                    return deferred
                return post

            def flush(i):
                T.dma('sp', lambda e: e.dma_start(out=dstT[:, :, bass.ts(i, TB * 128)].rearrange("h d t -> d h t"), in_=qst[:]),
                      reads=[pn + 'qst'])
            return post_factory, flush

        def mk_v(ctx, key, width, dst_rows, nsub=None, valid_src=None):
            s2 = ctx['s2']; pn = ctx['pname'] + key
            if nsub is None:
                vst = sb(s2, pn + "_vst", [128, TB, width], BF16)
            else:
                vst = sb(s2, pn + "_vst", [128, TB, 12, 129], BF16)
                vld = sb(s2, pn + "_vld", [128, TB], F32)
                T.op('pool', lambda g: g.memset(vst[:], 1.0), writes=[pn + 'vst'])
            state = {'first': True}

            def post_factory(coff, w, g=None):
                def post(pj_t, pjn, t, i):
                    if nsub is None:
                        T.op('act', lambda e: e.copy(out=vst[:, t, coff:coff + w], in_=pj_t[:, 0:w]), reads=[pjn], writes=[pn + 'vst'])
                    else:
                        T.op('act', lambda e: e.copy(out=vst[:, t, g * 4:(g + 1) * 4, 0:128], in_=pj_t[:, 0:512].rearrange("p (h d) -> p h d", h=4)), reads=[pjn], writes=[pn + 'vst'])
                return post

            def flush(i):
                if nsub is not None and valid_src is not None:
                    T.dma('sp', lambda e: e.dma_start(out=vld[:], in_=valid_src[bass.ts(i, 128), :]),
                          writes=[pn + 'vld'])
                    for t in range(TB):
                        T.op('dve', lambda e, t=t: e.tensor_copy(out=vst[:, t, :, 128], in_=vld[:, t:t + 1].to_broadcast([128, 12])),
                             reads=[pn + 'vld'], writes=[pn + 'vst'])
                if nsub is None:
                    T.dma('sp', lambda e: e.dma_start(out=dst_rows[bass.ts(i, TB * 128), :].rearrange("(t p) w -> p t w", p=128), in_=vst[:]),
                          reads=[pn + 'vst'])
                else:
                    T.dma('sp', lambda e: e.dma_start(out=dst_rows[bass.ts(i, TB * 128), :].rearrange("(t p) w -> p t w", p=128),
                                                      in_=vst[:].rearrange("p t a b -> p t (a b)")), reads=[pn + 'vst'])
            return post_factory, flush

        def mk_gate(ctx, key, dst_rows):
            s2 = ctx['s2']; pn = ctx['pname'] + key
            gst = sb(s2, pn + "_gst", [128, TB, 2048], BF16)

            def post_factory(coff):
                def post(pj_t, pjn, t, i):
                    T.op('act', lambda e: e.activation(out=gst[:, t, coff:coff + 512], in_=pj_t[:, 0:512], func=AF.Sigmoid), reads=[pjn], writes=[pn + 'gst'])
                return post

            def flush(i):
                T.dma('sp', lambda e: e.dma_start(out=dst_rows[bass.ts(i, TB * 128), :].rearrange("(t p) w -> p t w", p=128), in_=gst[:]),
                      reads=[pn + 'gst'])
            return post_factory, flush

        for ch in range(2):
            tok = slice(ch * OWN, (ch + 1) * OWN)
            if on('own1'):
                def mg(ctx, ch=ch, tok=tok):
                    pq, fq = mk_qk(ctx, 'q', 8, 0, 'axial', tA_o[tok, :], qaT[:, :, tok])
                    pk, fk = mk_qk(ctx, 'k', 2, 1, 'axial', tA_o[tok, :], kaT[ch][:, :, 0:OWN])
                    pv, fv = mk_v(ctx, 'v', 256, va[ch][0:OWN, :])
                    groups = [(0, 512, pq(0, 4)), (512, 512, pq(4, 4)), (1024, 256, pk(0, 2)), (1280, 256, pv(0, 256))]
                    return groups, (lambda i: (fq(i), fk(i), fv(i)))
                proj_pass("o1c%d" % ch, OWN, [(C_QA, 1536)], True, xo[tok, :], None, hT_own[:, :, tok], mg)
            if on('own1b'):
                def mg(ctx, ch=ch, tok=tok):
                    pv, fv = mk_v(ctx, 'v', 1536, vbx[ch][HALO:HALO + OWN, :], nsub=True)
                    return [(g * 512, 512, pv(0, 512, g)) for g in range(3)], fv
                proj_pass("o1b%d" % ch, OWN, [(C_VB, 1536)], False, None, hT_own[:, :, tok], None, mg)
            if on('own2'):
                def mg(ctx, ch=ch, tok=tok):
                    pq, fq = mk_qk(ctx, 'q', 12, 2, 'partial', tB_o[tok, :], qbT[:, :, tok])
                    return [(g * 512, 512, pq(g * 4, 4)) for g in range(3)], fq
                proj_pass("o2a%d" % ch, OWN, [(C_QB, 1536)], False, None, hT_own[:, :, tok], None, mg)

                def mg(ctx, ch=ch, tok=tok):
                    pk, fk = mk_qk(ctx, 'k', 12, 3, 'partial', tB_o[tok, :], kbT[ch][:, :, HALO:HALO + OWN])
                    return [(g * 512, 512, pk(g * 4, 4)) for g in range(3)], fk
                proj_pass("o2b%d" % ch, OWN, [(C_KB, 1536)], False, None, hT_own[:, :, tok], None, mg)
            if on('own3'):
                for gi_, c0 in enumerate((C_GA, C_GB)):
                    def mg(ctx, ch=ch, tok=tok, gi_=gi_):
                        pg, fg = mk_gate(ctx, 'g', sg[tok, gi_ * 2048:(gi_ + 1) * 2048])
                        return [(j * 512, 512, pg(j * 512)) for j in range(4)], fg
                    proj_pass("o3%d%d" % (gi_, ch), OWN, [(c0, 2048)], False, None, hT_own[:, :, tok], None, mg)
            if on('rest'):
                roff = 0 if ch == 0 else REST[0]
                rtok = slice(roff, roff + REST[ch])

                def mg(ctx, ch=ch, rtok=rtok):
                    pk, fk = mk_qk(ctx, 'k', 2, 1, 'axial', tA_r[rtok, :], kaT[ch][:, :, OWN:SEQS[ch]])
                    pv, fv = mk_v(ctx, 'v', 256, va[ch][OWN:SEQS[ch], :])
                    return [(0, 256, pk(0, 2)), (256, 256, pv(0, 256))], (lambda i: (fk(i), fv(i)))
                proj_pass("rs%d" % ch, REST[ch], [(C_KA, 512)], True, xr[rtok, :], None, None, mg)
            if on('halo'):
                for side in range(2):
                    hs = slice((ch * 2 + side) * HALO, (ch * 2 + side + 1) * HALO)
                    eo = 0 if side == 0 else HALO + OWN

                    def mg(ctx, ch=ch, hs=hs, eo=eo):
                        pk, fk = mk_qk(ctx, 'k', 12, 3, 'partial', tB_h[hs, :], kbT[ch][:, :, eo:eo + HALO])
                        return [(g * 512, 512, pk(g * 4, 4)) for g in range(3)], fk
                    proj_pass("hk%d%d" % (ch, side), HALO, [(C_KB, 1536)], True, xh[hs, :], None, None, mg)

                    def mg(ctx, ch=ch, hs=hs, eo=eo):
                        pv, fv = mk_v(ctx, 'v', 1536, vbx[ch][eo:eo + HALO, :], nsub=True, valid_src=vh[(ch * 2 + side) * (HALO // TB):(ch * 2 + side + 1) * (HALO // TB), :])
                        return [(g * 512, 512, pv(0, 512, g)) for g in range(3)], fv
                    proj_pass("hv%d%d" % (ch, side), HALO, [(C_VB, 1536)], True, xh[hs, :], None, None, mg)

        if on('attnA'):
            for ch in range(2):
                S = SEQS[ch]
                NKT = S // 128
                with ExitStack() as s2:
                    kt_sb = sb(s2, "A_kT%d" % ch, [128, 2, S], BF16)
                    v_sb = sb(s2, "A_v%d" % ch, [128, NKT, 256], BF16)
                    q_sb = sb(s2, "A_q%d" % ch, [128, 8, 128], BF16)
                    pT = [sb(s2, "A_pT%d%d" % (ch, k), [128, 512], BF16) for k in range(6)]
                    accD = [sb(s2, "A_accD%d%d" % (ch, k), [128, 512], F32) for k in range(2)]
                    accP = [sb(s2, "A_accP%d%d" % (ch, k), [128, 512], F32) for k in range(2)]
                    rd = sb(s2, "A_rd%d" % ch, [128, 512], F32)
                    ao = sb(s2, "A_ao%d" % ch, [128, 8, 128], BF16)
                    ps_s = [ps(s2, "A_pss%d%d" % (ch, k), [128, 512], F32) for k in range(3)]
                    ps_o = [ps(s2, "A_pso%d%d" % (ch, k), [128, 512], F32) for k in range(2)]
                    ps_d = [ps(s2, "A_psd%d%d" % (ch, k), [128, 512], F32) for k in range(2)]
                    for kv in range(2):
                        for c0 in range(0, S, min(S, 4096)):
                            T.dma('sp', lambda e, kv=kv, c0=c0: e.dma_start(out=kt_sb[:, kv, c0:c0 + min(S, 4096)], in_=kaT[ch][kv, :, c0:c0 + min(S, 4096)]), writes=['A_kT'])
                    VS = min(NKT, 32)
                    for c0 in range(0, NKT, VS):
                        T.dma('sp', lambda e, c0=c0: e.dma_start(out=v_sb[:, c0:c0 + VS, :],
                                                                 in_=va[ch][c0 * 128:(c0 + VS) * 128, :].rearrange("(t p) w -> p t w", p=128)), writes=['A_v'])
                    T.sync_all()
                    tok0 = ch * OWN
                    for i in range(OWN // 128):
                        T.dma('sp', lambda e: e.dma_start(out=q_sb[:], in_=qaT[:, :, tok0:tok0 + OWN][:, :, bass.ts(i, 128)].rearrange("h d t -> d h t")),
                              writes=['A_q'])
                        for kv in range(2):
                            q4 = q_sb[:, kv * 4:(kv + 1) * 4, :].rearrange("p h t -> p (h t)")
                            po, pd = ps_o[kv], ps_d[kv]

                            def s_mm(kt, kv=kv, q4=q4):
                                b = kt % 3
                                T.op('pe', lambda e: e.matmul(ps_s[b][:], lhsT=kt_sb[:, kv, kt * 128:(kt + 1) * 128], rhs=q4, start=True, stop=True),
                                     reads=['A_kT', 'A_q'], writes=['A_pss%d' % b])

                            accn = {'dve': 'A_accD%d' % kv, 'pool': 'A_accP%d' % kv}
                            acct = {'dve': accD[kv], 'pool': accP[kv]}
                            seen = set()

                            def rest_(kt, kv=kv, po=po, pd=pd):
                                b = kt % 3
                                pb = kt % 6
                                T.op('act', lambda e: e.activation(out=pT[pb][:], in_=ps_s[b][:], func=AF.Exp, bias=negC[:, 0:1], scale=SCALE),
                                     reads=['A_pss%d' % b, 'negC'], writes=['A_pT%d' % pb])
                                T.op('pe', lambda e: e.matmul(po[:], lhsT=v_sb[:, kt, kv * 128:(kv + 1) * 128], rhs=pT[pb][:], start=(kt == 0), stop=(kt == NKT - 1)),
                                     reads=['A_v', 'A_pT%d' % pb], writes=['A_pso%d' % kv])
                                en = 'pool' if kt % 2 == 1 else 'dve'
                                if en not in seen:
                                    seen.add(en)
                                    T.op(en, lambda e: e.tensor_copy(out=acct[en][:], in_=pT[pb][:]), reads=['A_pT%d' % pb], writes=[accn[en]])
                                else:
                                    T.op(en, lambda e: e.tensor_tensor(out=acct[en][:], in0=acct[en][:], in1=pT[pb][:], op=ALU.add),
                                         reads=['A_pT%d' % pb, accn[en]], writes=[accn[en]])
                            s_mm(0); s_mm(1)
                            for kt in range(NKT):
                                if kt + 2 < NKT:
                                    s_mm(kt + 2)
                                rest_(kt)
                            T.op('dve', lambda e: e.tensor_tensor(out=accD[kv][:], in0=accD[kv][:], in1=accP[kv][:], op=ALU.add),
                                 reads=[accn['dve'], accn['pool']], writes=[accn['dve']])
                            T.op('pe', lambda e, pd=pd: e.matmul(pd[:], lhsT=ones_f[:], rhs=accD[kv][:], start=True, stop=True),
                                 reads=['ones_f', accn['dve']], writes=['A_psd%d' % kv])
                            T.op('dve', lambda e, pd=pd: e.reciprocal(out=rd[:], in_=pd[:]), reads=['A_psd%d' % kv], writes=['A_rd'])
                            T.op('dve', lambda e, po=po, kv=kv: e.tensor_tensor(out=ao[:, kv * 4:(kv + 1) * 4, :].rearrange("p h t -> p (h t)"), in0=po[:], in1=rd[:], op=ALU.mult),
                                 reads=['A_pso%d' % kv, 'A_rd'], writes=['A_ao%d' % kv])
                        T.dma('sp', lambda e: e.dma_start(out=aoT[:, :, tok0:tok0 + OWN][:, :, bass.ts(i, 128)].rearrange("h d t -> d h t"), in_=ao[:]),
                              reads=['A_ao0', 'A_ao1'])
                        T.maybe_sync()
                    T.sync_all()

        if on('attnB'):
            with ExitStack() as s2:
                qg = sb(s2, "B_q", [128, 4, OWN], BF16)
                kg = sb(s2, "B_k", [128, 4, EXT], BF16)
                mA = sb(s2, "B_mA", [128, 4, 128], BF16)
                mB = sb(s2, "B_mB", [128, 4, 128], BF16)
                v1 = [sb(s2, "B_v%d" % k, [128, 4, 129], BF16) for k in range(4)]
                pA = [sb(s2, "B_pA%d" % k, [128, 4, 128], BF16) for k in range(2)]
                pB = [sb(s2, "B_pB%d" % k, [128, 4, 128], BF16) for k in range(2)]
                ob = [sb(s2, "B_ob%d" % k, [128, 4, 129], F32) for k in range(2)]
                psA = [ps(s2, "B_psA%d" % k, [128, 4, 128], F32) for k in range(2)]
                psB = [ps(s2, "B_psB%d" % k, [128, 4, 128], F32) for k in range(2)]
                po_ = [ps(s2, "B_po%d" % k, [128, 4, 256], F32) for k in range(1)]
                for m_, sgn in ((mA, 1), (mB, -1)):
                    nm = 'B_mA' if m_ is mA else 'B_mB'
                    T.op('pool', lambda g, m_=m_: g.memset(m_[:], 1.0), writes=[nm])
                    T.op('pool', lambda g, m_=m_, sgn=sgn: g.affine_select(out=m_[:], in_=m_[:], pattern=[[0, 4], [-sgn, 128]], compare_op=ALU.is_ge,
                                                                           fill=0.0, base=0, channel_multiplier=sgn), reads=[nm], writes=[nm])
                for ch in range(2):
                    tok0 = ch * OWN
                    for g in range(3):
                        r = B_DIL[g]
                        T.dma('sp', lambda e: e.dma_start(out=qg[:], in_=qbT[g * 4:(g + 1) * 4, :, tok0:tok0 + OWN].rearrange("h d t -> d h t")), writes=['B_q'])
                        T.dma('sp', lambda e: e.dma_start(out=kg[:], in_=kbT[ch][g * 4:(g + 1) * 4, :, :].rearrange("h d t -> d h t")), writes=['B_k'])
                        nqb = OWN // r // 128
                        vcnt = 0
                        it = 0
                        for c in range(r):
                            vt = {}
                            for qb in range(nqb):
                                u0 = qb * 128
                                for s_ in (qb, qb + 1):
                                    if s_ not in vt:
                                        k_ = vcnt % 4
                                        vcnt += 1
                                        vt[s_] = k_
                                        e0 = HALO + (128 * s_ - 64) * r + c
                                        T.dma('sp', lambda e, k_=k_, e0=e0: e.dma_start(
                                            out=v1[k_][:].rearrange("p h d -> p (h d)"),
                                            in_=vbx[ch][e0:e0 + 127 * r + 1:r, g * 516:(g + 1) * 516]), writes=['B_v%d' % k_])
                                kA, kB = vt[qb], vt[qb + 1]
                                b = it % 2
                                it += 1
                                qs = u0 * r + c
                                eA = HALO + (u0 - 64) * r + c
                                eB = HALO + (u0 + 64) * r + c
                                for h in range(4):
                                    T.op('pe', lambda e, h=h: e.matmul(psA[b][:, h, :], lhsT=kg[:, h, eA:eA + 127 * r + 1:r], rhs=qg[:, h, qs:qs + 127 * r + 1:r], start=True, stop=True),
                                         reads=['B_k', 'B_q'], writes=['B_psA%d' % b])
                                    T.op('pe', lambda e, h=h: e.matmul(psB[b][:, h, :], lhsT=kg[:, h, eB:eB + 127 * r + 1:r], rhs=qg[:, h, qs:qs + 127 * r + 1:r], start=True, stop=True),
                                         reads=['B_k', 'B_q'], writes=['B_psB%d' % b])
                                T.op('act', lambda e: e.activation(out=pA[b][:], in_=psA[b][:], func=AF.Exp, bias=negC[:, 1:2], scale=SCALE), reads=['B_psA%d' % b, 'negC'], writes=['B_pA%d' % b])
                                T.op('act', lambda e: e.activation(out=pB[b][:], in_=psB[b][:], func=AF.Exp, bias=negC[:, 1:2], scale=SCALE), reads=['B_psB%d' % b, 'negC'], writes=['B_pB%d' % b])
                                T.op('dve', lambda e: e.tensor_tensor(out=pA[b][:], in0=pA[b][:], in1=mA[:], op=ALU.mult), reads=['B_pA%d' % b, 'B_mA'], writes=['B_pA%d' % b])
                                T.op('pool', lambda e: e.tensor_tensor(out=pB[b][:], in0=pB[b][:], in1=mB[:], op=ALU.mult), reads=['B_pB%d' % b, 'B_mB'], writes=['B_pB%d' % b])
                                for h in range(4):
                                    T.op('pe', lambda e, h=h: e.matmul(po_[0][:, h, 0:129], lhsT=pA[b][:, h, :], rhs=v1[kA][:, h, :], start=True, stop=False),
                                         reads=['B_pA%d' % b, 'B_v%d' % kA], writes=['B_po'])
                                    T.op('pe', lambda e, h=h: e.matmul(po_[0][:, h, 0:129], lhsT=pB[b][:, h, :], rhs=v1[kB][:, h, :], start=False, stop=True),
                                         reads=['B_pB%d' % b, 'B_v%d' % kB], writes=['B_po'])
                                T.op('act', lambda e: e.copy(out=ob[b][:], in_=po_[0][:, :, 0:129]), reads=['B_po'], writes=['B_ob%d' % b])
                                t0 = tok0 + qs
                                T.dma('sp', lambda e: e.dma_start(out=bnum[g, t0:t0 + 127 * r + 1:r, :], in_=ob[b][:].rearrange("p h d -> p (h d)")), reads=['B_ob%d' % b])
                        T.sync_all()

        bc_reg = nc.gpsimd.alloc_register("bc")
        nc.gpsimd.reg_mov(bc_reg, XROWS - 1)
        if on('mix'):
            with ExitStack() as s2:
                woa = sb(s2, "M_woa", [128, 8, D], BF16)
                wob = sb(s2, "M_wob", [128, 4, D], BF16)
                wo = sb(s2, "M_wo", [128, KC, D], BF16)
                wr = sb(s2, "M_wr", [128, KC, 36], F32)
                br = sb(s2, "M_br", [128, 36], F32)
                ecap = sb(s2, "M_ecap", [128, NE], F32)
                base = sb(s2, "M_base", [128, NE], F32)
                utri = sb(s2, "M_utri", [128, 128], BF16)
                aot = sb(s2, "M_aot", [128, 8, 128], BF16)
                bn = sb(s2, "M_bn", [128, 3, 516], F32)
                bs = sb(s2, "M_bs", [128, 4, 129], F32)
                rdn = sb(s2, "M_rdn", [128, 4], F32)
                bo = sb(s2, "M_bo", [128, 4, 128], BF16)
                boT = sb(s2, "M_boT", [128, 4, 128], BF16)
                sgt = sb(s2, "M_sg", [128, 4096], BF16)
                xt = sb(s2, "M_xt", [128, D], F32)
                m1 = sb(s2, "M_m1", [128, 512], F32)
                m2 = sb(s2, "M_m2", [128, 512], F32)
                mixed = sb(s2, "M_mixed", [128, D], BF16)
                mixT = sb(s2, "M_mixT", [128, KC, 128], BF16)
                xm = sb(s2, "M_xm", [128, D], F32)
                gffn_b = sb(s2, "M_gffn", [128, D], F32)
                T.dma('sp', lambda e: e.dma_start(out=gffn_b[:], in_=g_ffn.to_broadcast([128, D])), writes=['gffn_b'])
                ssq = sb(s2, "M_ssq", [128, 1], F32)
                h2f = sb(s2, "M_h2f", [128, D], F32)
                h2b = sb(s2, "M_h2b", [128, D], BF16)
                h2T = sb(s2, "M_h2T", [128, 8, 128], F32)
                lg = sb(s2, "M_lg", [128, 36], F32)
                sm = sb(s2, "M_sm", [128, 16], F32)
                ohg = sb(s2, "M_ohg", [128, 4], F32)
                les = sb(s2, "M_les", [128, 4, 8], F32)
                lsel = sb(s2, "M_lsel", [128, 8], F32)
                oh1 = sb(s2, "M_oh1", [128, 8], F32)
                oh2 = sb(s2, "M_oh2", [128, 8], F32)
                msk = sb(s2, "M_msk", [128, 8], F32)
                o32 = sb(s2, "M_o32", [128, 2, 4, 8], F32)
                Mb = sb(s2, "M_Mb", [128, NE], BF16)
                tot = sb(s2, "M_tot", [128, NE], F32)
                rk = sb(s2, "M_rk", [128, NE], F32)
                tmp32 = sb(s2, "M_tmp32", [128, NE], F32)
                dst_f = sb(s2, "M_dstf", [128, 2], F32)
                rk_f = sb(s2, "M_rkf", [128, 2], F32)
                dst_i = sb(s2, "M_dsti", [128, 2], I32)
                wts = sb(s2, "M_wts", [128, 2], F32)
                pj = [ps(s2, "M_pj%d" % k, [128, 512], F32) for k in range(3)]
                ptb = ps(s2, "M_ptb", [128, KC, 128], BF16)
                ptf = ps(s2, "M_ptf", [128, 8, 128], F32)
                plog = ps(s2, "M_plog", [128, 64], F32)
                for kc in range(0, 8, 4):
                    T.dma('pool', lambda g, kc=kc: g.dma_start(out=woa[:, kc:kc + 4, :], in_=w_oa[kc * 128:(kc + 4) * 128, :].rearrange("(c p) n -> p c n", p=128)), writes=['M_w'])
                T.dma('pool', lambda g: g.dma_start(out=wob[:], in_=w_ob.rearrange("(c p) n -> p c n", p=128)), writes=['M_w'])
                for kc in range(0, KC, 4):
                    T.dma('pool', lambda g, kc=kc: g.dma_start(out=wo[:, kc:kc + 4, :], in_=w_out[kc * 128:(kc + 4) * 128, :].rearrange("(c p) n -> p c n", p=128)), writes=['M_w'])
                T.dma('sp', lambda e: e.dma_start(out=wr[:], in_=w_r.rearrange("(c p) n -> p c n", p=128)), writes=['M_w'])
                T.dma('sp', lambda e: e.dma_start(out=br[:], in_=b_r.to_broadcast([128, 36])), writes=['M_w'])
                T.op('pool', lambda g: g.iota(ecap[:], pattern=[[CAP, NE]], base=0, channel_multiplier=0, allow_small_or_imprecise_dtypes=True), writes=['M_w'])
                T.op('pool', lambda g: g.memset(base[:], 0.0), writes=['M_base'])
                T.op('pool', lambda g: g.memset(h2f[:], 0.0), writes=['h2f'])
                h2fz = h2f[:].bitcast(BF16)
                for r0 in range(0, XROWS, 256):
                    T.dma('sp', lambda e, r0=r0: e.dma_start(out=xin[r0:r0 + 256, :].rearrange("(p a) w -> p (a w)", p=128), in_=h2fz), reads=['h2f'], writes=['xin'])
                T.op('pool', lambda g: g.memset(utri[:], 1.0), writes=['M_utri'])
                T.op('pool', lambda g: g.affine_select(out=utri[:], in_=utri[:], pattern=[[1, 128]], compare_op=ALU.is_ge, fill=0.0, base=-1, channel_multiplier=-1),
                     reads=['M_utri'], writes=['M_utri'])
                T.sync_all()
                for i in range(NT_OWN):
                    rows = bass.ts(i, 128)
                    T.dma('sp', lambda e: e.dma_start(out=aot[:], in_=aoT[:, :, rows].rearrange("h d t -> d h t")), writes=['aot'])
                    T.dma('sp', lambda e: e.dma_start(out=bn[:], in_=bnum[:, rows, :].rearrange("g t w -> t g w")), writes=['bn'])
                    T.dma('sp', lambda e: e.dma_start(out=sgt[:], in_=sg[rows, :]), writes=['sgt'])
                    T.dma('sp', lambda e: e.dma_start(out=xt[:], in_=xo[rows, :]), writes=['xt'])
                    bs2 = bs[:].rearrange("p h d -> p (h d)")
                    T.op('dve', lambda e: e.tensor_tensor(out=bs2, in0=bn[:, 0, :], in1=bn[:, 1, :], op=ALU.add), reads=['bn'], writes=['bs'])
                    T.op('dve', lambda e: e.tensor_tensor(out=bs2, in0=bs2, in1=bn[:, 2, :], op=ALU.add), reads=['bn', 'bs'], writes=['bs'])
                    T.op('dve', lambda e: e.reciprocal(out=rdn[:], in_=bs[:, :, 128]), reads=['bs'], writes=['rdn'])
                    T.op('dve', lambda e: e.tensor_tensor(out=bo[:], in0=bs[:, :, 0:128], in1=rdn[:].unsqueeze(2).to_broadcast([128, 4, 128]), op=ALU.mult),
                         reads=['bs', 'rdn'], writes=['bo'])
                    for h in range(4):
                        T.op('pe', lambda e, h=h: e.transpose(out=ptb[:, h, :], in_=bo[:, h, :], identity=ident_b[:]), reads=['bo', 'ident_b'], writes=['ptb'])
                    T.op('act', lambda e: e.copy(out=boT[:], in_=ptb[:, 0:4, :]), reads=['ptb'], writes=['boT'])
                    for j in range(4):
                        cs = slice(j * 512, (j + 1) * 512)
                        pa, pb_ = pj[0], pj[1]
                        for h in range(8):
                            T.op('pe', lambda e, h=h: e.matmul(pa[:], lhsT=aot[:, h, :], rhs=woa[:, h, cs], start=(h == 0), stop=(h == 7)), reads=['aot', 'M_w'], writes=['pj0'])
                        for h in range(4):
                            T.op('pe', lambda e, h=h: e.matmul(pb_[:], lhsT=boT[:, h, :], rhs=wob[:, h, cs], start=(h == 0), stop=(h == 3)), reads=['boT', 'M_w'], writes=['pj1'])
                        T.op('dve', lambda e: e.tensor_tensor(out=m1[:], in0=pa[:], in1=sgt[:, j * 512:(j + 1) * 512], op=ALU.mult), reads=['pj0', 'sgt'], writes=['m1'])
                        T.op('dve', lambda e: e.tensor_tensor(out=m2[:], in0=pb_[:], in1=sgt[:, 2048 + j * 512:2048 + (j + 1) * 512], op=ALU.mult), reads=['pj1', 'sgt'], writes=['m2'])
                        T.op('pool', lambda e: e.tensor_tensor(out=mixed[:, cs], in0=m1[:], in1=m2[:], op=ALU.add), reads=['m1', 'm2'], writes=['mixed'])
                    for c in range(KC):
                        T.op('pe', lambda e, c=c: e.transpose(out=ptb[:, c, :], in_=mixed[:, c * 128:(c + 1) * 128], identity=ident_b[:]), reads=['mixed', 'ident_b'], writes=['ptb'])
                    T.op('act', lambda e: e.copy(out=mixT[:, 0:8, :], in_=ptb[:, 0:8, :]), reads=['ptb'], writes=['mixTa'])
                    T.op('dve', lambda e: e.tensor_copy(out=mixT[:, 8:16, :], in_=ptb[:, 8:16, :]), reads=['ptb'], writes=['mixTb'])
                    for j in range(4):
                        cs = slice(j * 512, (j + 1) * 512)
                        pbk = pj[2] if j % 2 == 0 else pj[0]
                        pbn = 'pj2' if j % 2 == 0 else 'pj0'
                        for c in range(KC):
                            T.op('pe', lambda e, c=c: e.matmul(pbk[:], lhsT=mixT[:, c, :], rhs=wo[:, c, cs], start=(c == 0), stop=(c == KC - 1)), reads=['mixTa', 'mixTb', 'M_w'], writes=[pbn])
                        T.op('dve', lambda e: e.tensor_tensor(out=xm[:, cs], in0=pbk[:], in1=xt[:, cs], op=ALU.add), reads=[pbn, 'xt'], writes=['xm'])
                    T.dma('sp', lambda e: e.dma_start(out=x_mid[rows, :], in_=xm[:]), reads=['xm'])
                    T.op('pool', lambda e: e.memset(ssq[:], 0.0), writes=['ssq'])
                    T.op('act', lambda e: e.activation(out=mixed[:], in_=xm[:], func=AF.Square, accum_out=ssq[:]), reads=['xm', 'ssq'], writes=['mixed', 'ssq'])
                    T.op('act', lambda e: e.activation(out=ssq[:], in_=ssq[:], func=AF.Sqrt, bias=epsb[:], scale=1.0 / D), reads=['ssq', 'epsb'], writes=['ssq'])
                    T.op('dve', lambda e: e.reciprocal(out=ssq[:], in_=ssq[:]), reads=['ssq'], writes=['ssq'])
                    T.op('dve', lambda e: e.scalar_tensor_tensor(out=h2f[:], in0=xm[:], scalar=ssq[:], in1=gffn_b[:], op0=ALU.mult, op1=ALU.mult), reads=['xm', 'ssq', 'gffn_b'], writes=['h2f'])
                    T.op('act', lambda e: e.copy(out=h2b[:], in_=h2f[:]), reads=['h2f'], writes=['h2b'])
                    T.dma('sp', lambda e: e.dma_start(out=h2[rows, :], in_=h2b[:]), reads=['h2b'])
                    for half in range(2):
                        for c in range(8):
                            cc = half * 8 + c
                            T.op('pe', lambda e, c=c, cc=cc: e.transpose(out=ptf[:, c, :], in_=h2f[:, cc * 128:(cc + 1) * 128], identity=ident_f[:]), reads=['h2f', 'ident_f'], writes=['ptf'])
                        T.op('act', lambda e: e.copy(out=h2T[:, 0:4, :], in_=ptf[:, 0:4, :]), reads=['ptf'], writes=['h2Ta'])
                        T.op('dve', lambda e: e.tensor_copy(out=h2T[:, 4:8, :], in_=ptf[:, 4:8, :]), reads=['ptf'], writes=['h2Tb'])
                        for c in range(8):
                            cc = half * 8 + c
                            T.op('pe', lambda e, c=c, cc=cc: e.matmul(plog[:, 0:36], lhsT=h2T[:, c, :], rhs=wr[:, cc, :], start=(cc == 0), stop=(cc == KC - 1)),
                                 reads=['h2Ta', 'h2Tb', 'M_w'], writes=['plog'])
                    T.op('dve', lambda e: e.tensor_tensor(out=lg[:], in0=plog[:, 0:36], in1=br[:], op=ALU.add), reads=['plog', 'M_w'], writes=['lg'])
                    V = lambda e: e
                    T.op('dve', lambda e: e.tensor_reduce(out=sm[:, 0:1], in_=lg[:, 0:4], axis=AX.X, op=ALU.max), reads=['lg'], writes=['sm0'])
                    T.op('dve', lambda e: e.tensor_scalar(out=ohg[:], in0=lg[:, 0:4], scalar1=sm[:, 0:1], scalar2=None, op0=ALU.is_equal), reads=['lg', 'sm0'], writes=['ohg'])
                    T.op('dve', lambda e: e.tensor_scalar(out=sm[:, 1:2], in0=sm[:, 0:1], scalar1=-1.0, scalar2=None, op0=ALU.mult), reads=['sm0'], writes=['sm1'])
                    T.op('pool', lambda e: e.memset(sm[:, 2:3], 0.0), writes=['sm2'])
                    T.op('act', lambda e: e.activation(out=sm[:, 4:8], in_=lg[:, 0:4], func=AF.Exp, bias=sm[:, 1:2], scale=1.0, accum_out=sm[:, 2:3]), reads=['lg', 'sm1', 'sm2'], writes=['sm2', 'sm4'])
                    T.op('dve', lambda e: e.reciprocal(out=sm[:, 3:4], in_=sm[:, 2:3]), reads=['sm2'], writes=['sm3'])
                    T.op('dve', lambda e: e.tensor_tensor(out=les[:], in0=lg[:, 4:36].rearrange("p (g x) -> p g x", g=4), in1=ohg[:].unsqueeze(2).to_broadcast([128, 4, 8]), op=ALU.mult),
                         reads=['lg', 'ohg'], writes=['les'])
                    T.op('dve', lambda e: e.tensor_reduce(out=lsel[:], in_=les[:].rearrange("p g x -> p x g"), axis=AX.X, op=ALU.add), reads=['les'], writes=['lsel'])
                    T.op('dve', lambda e: e.tensor_reduce(out=sm[:, 8:9], in_=lsel[:], axis=AX.X, op=ALU.max), reads=['lsel'], writes=['sm8'])
                    T.op('dve', lambda e: e.tensor_scalar(out=oh1[:], in0=lsel[:], scalar1=sm[:, 8:9], scalar2=None, op0=ALU.is_equal), reads=['lsel', 'sm8'], writes=['oh1'])
                    T.op('dve', lambda e: e.scalar_tensor_tensor(out=msk[:], in0=oh1[:], scalar=-1e30, in1=lsel[:], op0=ALU.mult, op1=ALU.add), reads=['oh1', 'lsel'], writes=['msk'])
                    T.op('dve', lambda e: e.tensor_reduce(out=sm[:, 9:10], in_=msk[:], axis=AX.X, op=ALU.max), reads=['msk'], writes=['sm9'])
                    T.op('dve', lambda e: e.tensor_scalar(out=oh2[:], in0=msk[:], scalar1=sm[:, 9:10], scalar2=None, op0=ALU.is_equal), reads=['msk', 'sm9'], writes=['oh2'])
                    T.op('dve', lambda e: e.tensor_tensor(out=sm[:, 10:11], in0=sm[:, 9:10], in1=sm[:, 8:9], op=ALU.subtract), reads=['sm8', 'sm9'], writes=['sm10'])
                    T.op('act', lambda e: e.activation(out=sm[:, 11:12], in_=sm[:, 10:11], func=AF.Exp), reads=['sm10'], writes=['sm11'])
                    T.op('dve', lambda e: e.tensor_scalar(out=sm[:, 12:13], in0=sm[:, 11:12], scalar1=1.0, scalar2=None, op0=ALU.add), reads=['sm11'], writes=['sm12'])
                    T.op('dve', lambda e: e.reciprocal(out=sm[:, 13:14], in_=sm[:, 12:13]), reads=['sm12'], writes=['sm13'])
                    T.op('dve', lambda e: e.tensor_tensor(out=wts[:, 0:1], in0=sm[:, 13:14], in1=sm[:, 3:4], op=ALU.mult), reads=['sm13', 'sm3'], writes=['wts'])
                    T.op('dve', lambda e: e.tensor_tensor(out=wts[:, 1:2], in0=wts[:, 0:1], in1=sm[:, 11:12], op=ALU.mult), reads=['wts', 'sm11'], writes=['wts'])
                    for k_, ohk in enumerate((oh1, oh2)):
                        T.op('dve', lambda e, k_=k_, ohk=ohk: e.tensor_tensor(out=o32[:, k_, :, :], in0=ohg[:].unsqueeze(2).to_broadcast([128, 4, 8]),
                                                                              in1=ohk[:].unsqueeze(1).to_broadcast([128, 4, 8]), op=ALU.mult), reads=['ohg', 'oh1', 'oh2'], writes=['o32_%d' % k_])
                    o32f = o32[:].rearrange("p k g x -> p k (g x)")
                    T.op('dve', lambda e: e.tensor_tensor(out=Mb[:], in0=o32f[:, 0, :], in1=o32f[:, 1, :], op=ALU.add), reads=['o32_0', 'o32_1'], writes=['Mb'])
                    T.op('pe', lambda e: e.matmul(plog[:, 0:32], lhsT=utri[:], rhs=Mb[:], start=True, stop=True), reads=['Mb', 'M_utri', 'lg'], writes=['plog'])
                    T.op('dve', lambda e: e.tensor_tensor(out=rk[:], in0=plog[:, 0:32], in1=base[:], op=ALU.add), reads=['plog', 'M_base'], writes=['rk'])
                    T.op('pe', lambda e: e.matmul(plog[:, 32:64], lhsT=ones_b[:], rhs=Mb[:], start=True, stop=True), reads=['Mb', 'ones_b'], writes=['plog'])
                    T.op('dve', lambda e: e.tensor_tensor(out=base[:], in0=plog[:, 32:64], in1=base[:], op=ALU.add), reads=['plog', 'M_base', 'rk'], writes=['M_base'])
                    T.op('dve', lambda e: e.tensor_tensor(out=tot[:], in0=rk[:], in1=ecap[:], op=ALU.add), reads=['rk', 'M_w'], writes=['tot'])
                    for k_ in range(2):
                        T.op('dve', lambda e, k_=k_: e.tensor_tensor(out=tmp32[:], in0=o32f[:, k_, :], in1=tot[:], op=ALU.mult), reads=['o32_%d' % k_, 'tot'], writes=['tmp32'])
                        T.op('dve', lambda e, k_=k_: e.tensor_reduce(out=dst_f[:, k_:k_ + 1], in_=tmp32[:], axis=AX.X, op=ALU.add), reads=['tmp32'], writes=['dstf'])
                        T.op('dve', lambda e, k_=k_: e.tensor_tensor(out=tmp32[:], in0=o32f[:, k_, :], in1=rk[:], op=ALU.mult), reads=['o32_%d' % k_, 'rk', 'dstf'], writes=['tmp32'])
                        T.op('dve', lambda e, k_=k_: e.tensor_reduce(out=rk_f[:, k_:k_ + 1], in_=tmp32[:], axis=AX.X, op=ALU.add), reads=['tmp32'], writes=['rkf'])
                    T.op('dve', lambda e: e.tensor_scalar(out=rk_f[:], in0=rk_f[:], scalar1=float(CAP), scalar2=None, op0=ALU.is_ge), reads=['rkf'], writes=['rkf'])
                    T.op('dve', lambda e: e.scalar_tensor_tensor(out=dst_f[:], in0=rk_f[:], scalar=1.0e6, in1=dst_f[:], op0=ALU.mult, op1=ALU.add), reads=['rkf', 'dstf'], writes=['dstf'])
                    T.op('dve', lambda e: e.tensor_scalar(out=rk_f[:], in0=rk_f[:], scalar1=-1.0, scalar2=1.0, op0=ALU.mult, op1=ALU.add), reads=['rkf', 'dstf'], writes=['rkf'])
                    T.op('dve', lambda e: e.tensor_tensor(out=wts[:], in0=wts[:], in1=rk_f[:], op=ALU.mult), reads=['rkf', 'wts'], writes=['wts'])
                    T.op('dve', lambda e: e.tensor_copy(out=dst_i[:], in_=dst_f[:]), reads=['dstf'], writes=['dsti'])
                    T.dma('sp', lambda e: e.dma_start(out=dest_d[rows, :], in_=dst_i[:]), reads=['dsti'])
                    T.dma('sp', lambda e: e.dma_start(out=wts_d[rows, :], in_=wts[:]), reads=['wts'])
                    for k_ in range(2):
                        T.dma('pool', lambda g, k_=k_: g.indirect_dma_start(out=xin[:, :], out_offset=bass.IndirectOffsetOnAxis(ap=dst_i[:, k_:k_ + 1], axis=0),
                                                                           in_=h2b[:], in_offset=None, bounds_check=bc_reg, oob_is_err=False),
                              reads=['dsti', 'h2b', 'xin'])
                    T.maybe_sync()
                T.sync_all()

        if on('experts'):
            with ExitStack() as s2:
                NB = CAP // 128
                w1s = [sb(s2, "E_w1_%d" % k, [128, KC, DE], BF16) for k in range(2)]
                w3s = [sb(s2, "E_w3_%d" % k, [128, KC, DE], BF16) for k in range(2)]
                w2s = [sb(s2, "E_w2_%d" % k, [128, 4, D], BF16) for k in range(2)]
                xb_ = [sb(s2, "E_xb%d" % k, [128, D], BF16) for k in range(2)]
                xT = sb(s2, "E_xT", [128, KC, CAP], BF16)
                sl = sb(s2, "E_sl", [128, 512], F32)
                hT = sb(s2, "E_hT", [128, 4, CAP], BF16)
                ost = [sb(s2, "E_ost%d" % k, [128, D], BF16) for k in range(2)]
                ptb = ps(s2, "E_ptb", [128, KC, 128], BF16)
                p1 = ps(s2, "E_p1", [128, 512], F32)
                p3 = ps(s2, "E_p3", [128, 512], F32)
                po = [ps(s2, "E_po%d" % k, [128, 512], F32) for k in range(2)]

                def load_w(e_):
                    k = e_ % 2
                    for kc in range(0, KC, 8):
                        T.dma('pool', lambda g, kc=kc: g.dma_start(out=w1s[k][:, kc:kc + 8, :], in_=w1[e_, kc * 128:(kc + 8) * 128, :].rearrange("(c p) n -> p c n", p=128)), writes=['E_w1_%d' % k])
                        T.dma('pool', lambda g, kc=kc: g.dma_start(out=w3s[k][:, kc:kc + 8, :], in_=w3[e_, kc * 128:(kc + 8) * 128, :].rearrange("(c p) n -> p c n", p=128)), writes=['E_w3_%d' % k])
                    for kc in range(0, 4, 2):
                        T.dma('pool', lambda g, kc=kc: g.dma_start(out=w2s[k][:, kc:kc + 2, :], in_=w2[e_, kc * 128:(kc + 2) * 128, :].rearrange("(c p) n -> p c n", p=128)), writes=['E_w2_%d' % k])

                load_w(0)
                for ei in range(NE):
                    if ei + 1 < NE:
                        load_w(ei + 1)
                    wk = ei % 2
                    W1, W3, W2 = w1s[wk], w3s[wk], w2s[wk]
                    n1, n3, n2 = 'E_w1_%d' % wk, 'E_w3_%d' % wk, 'E_w2_%d' % wk
                    xrows = xin[bass.ts(ei, CAP), :]
                    orows = eout[bass.ts(ei, CAP), :]
                    for b in range(NB):
                        k = b % 2
                        T.dma('sp', lambda e, b=b, k=k: e.dma_start(out=xb_[k][:], in_=xrows[b * 128:(b + 1) * 128, :]), writes=['E_xb%d' % k])
                        for c in range(KC):
                            T.op('pe', lambda e, c=c, k=k: e.transpose(out=ptb[:, c, :], in_=xb_[k][:, c * 128:(c + 1) * 128], identity=ident_b[:]),
                                 reads=['E_xb%d' % k, 'ident_b'], writes=['E_ptb%d' % (c // 8)])
                        T.op('act', lambda e, b=b: e.copy(out=xT[:, 0:8, b * 128:(b + 1) * 128], in_=ptb[:, 0:8, :]), reads=['E_ptb0'], writes=['E_xT%d' % b])
                        T.op('dve', lambda e, b=b: e.tensor_copy(out=xT[:, 8:16, b * 128:(b + 1) * 128], in_=ptb[:, 8:16, :]), reads=['E_ptb1'], writes=['E_xTb%d' % b])
                    xres = ['E_xT%d' % b for b in range(NB)] + ['E_xTb%d' % b for b in range(NB)]
                    for n0 in range(0, CAP, 512):
                        nn = min(512, CAP - n0)
                        for fb in range(4):
                            for c in range(KC):
                                T.op('pe', lambda e, c=c, fb=fb: e.matmul(p1[:, 0:nn], lhsT=W1[:, c, fb * 128:(fb + 1) * 128], rhs=xT[:, c, n0:n0 + nn], start=(c == 0), stop=(c == KC - 1)),
                                     reads=xres + [n1], writes=['E_p1'])
                            for c in range(KC):
                                T.op('pe', lambda e, c=c, fb=fb: e.matmul(p3[:, 0:nn], lhsT=W3[:, c, fb * 128:(fb + 1) * 128], rhs=xT[:, c, n0:n0 + nn], start=(c == 0), stop=(c == KC - 1)),
                                     reads=xres + [n3], writes=['E_p3'])
                            T.op('act', lambda e: e.activation(out=sl[:, 0:nn], in_=p1[:, 0:nn], func=AF.Silu), reads=['E_p1'], writes=['E_sl'])
                            T.op('dve', lambda e, fb=fb: e.tensor_tensor(out=hT[:, fb, n0:n0 + nn], in0=p3[:, 0:nn], in1=sl[:, 0:nn], op=ALU.mult), reads=['E_p3', 'E_sl'], writes=['E_hT'])
                    for b in range(NB):
                        k = b % 2
                        for j in range(4):
                            pk = po[j % 2]
                            for fb in range(4):
                                T.op('pe', lambda e, fb=fb, b=b, j=j: e.matmul(pk[:], lhsT=hT[:, fb, b * 128:(b + 1) * 128], rhs=W2[:, fb, j * 512:(j + 1) * 512], start=(fb == 0), stop=(fb == 3)),
                                     reads=['E_hT', n2], writes=['E_po%d' % (j % 2)])
                            if j % 2 == 0:
                                T.op('act', lambda e, j=j, k=k: e.copy(out=ost[k][:, j * 512:(j + 1) * 512], in_=pk[:]), reads=['E_po0'], writes=['E_ost%d' % k])
                            else:
                                T.op('dve', lambda e, j=j, k=k: e.tensor_copy(out=ost[k][:, j * 512:(j + 1) * 512], in_=pk[:]), reads=['E_po1'], writes=['E_ost%d' % k])
                        T.dma('sp', lambda e, b=b, k=k: e.dma_start(out=orows[b * 128:(b + 1) * 128, :], in_=ost[k][:]), reads=['E_ost%d' % k])
                    T.maybe_sync()
                T.sync_all()

        if on('combine'):
            with ExitStack() as s2:
                g0 = sb(s2, "C_g0", [128, D], BF16)
                g1 = sb(s2, "C_g1", [128, D], BF16)
                xm = sb(s2, "C_xm", [128, D], F32)
                acc = sb(s2, "C_acc", [128, D], F32)
                di = sb(s2, "C_di", [128, 2], I32)
                wv = sb(s2, "C_wv", [128, 2], F32)
                T.op('pool', lambda g: g.memset(g0[:], 0.0), writes=['C_g0'])
                T.op('pool', lambda g: g.memset(g1[:], 0.0), writes=['C_g1'])
                T.sync_all()
                for i in range(NT_OWN):
                    rows = bass.ts(i, 128)
                    T.dma('sp', lambda e: e.dma_start(out=di[:], in_=dest_d[rows, :]), writes=['C_di'])
                    T.dma('sp', lambda e: e.dma_start(out=wv[:], in_=wts_d[rows, :]), writes=['C_wv'])
                    T.dma('sp', lambda e: e.dma_start(out=xm[:], in_=x_mid[rows, :]), writes=['C_xm'])
                    for k_, gk in enumerate((g0, g1)):
                        T.dma('pool', lambda g, k_=k_, gk=gk: g.indirect_dma_start(out=gk[:], out_offset=None, in_=eout[:, :],
                                                                                  in_offset=bass.IndirectOffsetOnAxis(ap=di[:, k_:k_ + 1], axis=0),
                                                                                  bounds_check=bc_reg, oob_is_err=False), reads=['C_di'], writes=['C_g%d' % k_])
                    T.op('dve', lambda e: e.scalar_tensor_tensor(out=acc[:], in0=g0[:], scalar=wv[:, 0:1], in1=xm[:], op0=ALU.mult, op1=ALU.add), reads=['C_g0', 'C_wv', 'C_xm'], writes=['C_acc'])
                    T.op('dve', lambda e: e.scalar_tensor_tensor(out=acc[:], in0=g1[:], scalar=wv[:, 1:2], in1=acc[:], op0=ALU.mult, op1=ALU.add), reads=['C_g1', 'C_wv', 'C_acc'], writes=['C_acc'])
                    T.dma('sp', lambda e: e.dma_start(out=y[rows, :], in_=acc[:]), reads=['C_acc'])
                    T.maybe_sync()
                T.sync_all()
        T.sync_all()
    return nc, in_names


def _tabA(pos):
    j = np.arange(32, dtype=np.float32)
    inv = np.power(np.float32(10000.0), -j / np.float32(32)).astype(np.float32)
    r = (pos // 64).astype(np.float32)[:, None] * inv[None, :]
    c = (pos % 64).astype(np.float32)[:, None] * inv[None, :]
    return np.concatenate([np.cos(r), np.cos(c), np.sin(r), np.sin(c)], axis=1).astype(np.float32)


def _tabB(pos):
    j = np.arange(16, dtype=np.float32)
    inv = np.power(np.float32(500000.0), -j / np.float32(16)).astype(np.float32)
    a = pos.astype(np.float32)[:, None] * inv[None, :]
    return np.concatenate([np.cos(a), np.sin(a)], axis=1).astype(np.float32)


def make_in_maps(inputs, OWN):
    xp, xs = np.asarray(inputs['x_prompt']), np.asarray(inputs['x_sample'])
    shared = {
        'g_mix': np.ascontiguousarray(inputs['g_mix'][0][None, :]), 'g_ffn': np.ascontiguousarray(inputs['g_ffn'][0][None, :]),
        'g_qa': np.ascontiguousarray(inputs['g_qa'][0][None, :]), 'g_ka': np.ascontiguousarray(inputs['g_ka'][0][None, :]),
        'g_qb': np.ascontiguousarray(inputs['g_qb'][0][None, :]), 'g_kb': np.ascontiguousarray(inputs['g_kb'][0][None, :]),
        'w_in': np.ascontiguousarray(inputs['w_in'][0]), 'w_oa': np.ascontiguousarray(inputs['w_oa'][0]),
        'w_ob': np.ascontiguousarray(inputs['w_ob'][0]), 'w_out': np.ascontiguousarray(inputs['w_out'][0]),
        'w_r': np.ascontiguousarray(np.concatenate([inputs['w_rg'][0], inputs['w_re'][0]], axis=1)),
        'b_r': np.ascontiguousarray(np.concatenate([inputs['b_rg'][0], inputs['b_re'][0]])[None, :]),
        'w1': np.ascontiguousarray(inputs['w1'][0]), 'w3': np.ascontiguousarray(inputs['w3'][0]), 'w2': np.ascontiguousarray(inputs['w2'][0]),
    }
    maps = []
    for c in range(8):
        chunks = [(xp[c // 2], (c % 2) * OWN), (xs[c // 4], (c % 4) * OWN)]
        xo, xr, xh, tAo, tBo, tAr, tBh, vh = [], [], [], [], [], [], [], []
        for (seq, p0) in chunks:
            S = seq.shape[0]
            own = np.arange(p0, p0 + OWN)
            rest = np.concatenate([np.arange(0, p0), np.arange(p0 + OWN, S)])
            xo.append(seq[own]); tAo.append(_tabA(own)); tBo.append(_tabB(own))
            xr.append(seq[rest]); tAr.append(_tabA(rest))
            for hp in (np.arange(p0 - HALO, p0), np.arange(p0 + OWN, p0 + OWN + HALO)):
                ok = (hp >= 0) & (hp < S)
                blk = np.zeros((HALO, D), np.float32)
                blk[ok] = seq[hp[ok]]
                xh.append(blk); tBh.append(_tabB(np.clip(hp, 0, S - 1))); vh.append(ok.astype(np.float32)[:, None])
        m = dict(shared)
        m.update(xo=np.concatenate(xo), xr=np.concatenate(xr), xh=np.concatenate(xh), tA_o=np.concatenate(tAo), tB_o=np.concatenate(tBo),
                 tA_r=np.concatenate(tAr), tB_h=np.concatenate(tBh),
                 vh=np.concatenate(vh).reshape(-1, TB, 128).transpose(0, 2, 1).reshape(-1, TB))
        maps.append({k: np.ascontiguousarray(v) for k, v in m.items()})
    return maps


def run(inputs, OWN, CAP, stages=None, dbg=(), cores=8):
    nc, in_names = build(OWN, CAP, stages=stages, dbg=dbg)
    maps = [{k: m[k] for k in in_names} for m in make_in_maps(inputs, OWN)[:cores]]
    res = run_bass_kernel_spmd(nc, maps, core_ids=list(range(cores)))
    return res.results


def kernel(**inputs):
    OWN = 4096
    res = run(inputs, OWN, 640)
    yp = np.empty((4, 8192, D), np.float32)
    ys = np.empty((2, 16384, D), np.float32)
    for c in range(8):
        yc = res[c]['y']
        yp[c // 2, (c % 2) * OWN:(c % 2 + 1) * OWN] = yc[:OWN]
        ys[c // 4, (c % 4) * OWN:(c % 4 + 1) * OWN] = yc[OWN:]
    return (yp, ys)
```

```python
import math
import numpy as np
from contextlib import ExitStack
import concourse.bass as bass
import concourse.mybir as mybir
from concourse.bass_utils import run_bass_kernel_spmd

F32 = mybir.dt.float32
BF16 = mybir.dt.bfloat16
I32 = mybir.dt.int32
ALU = mybir.AluOpType
AF = mybir.ActivationFunctionType
AX = mybir.AxisListType

D = 2048
KC = 16
HD = 128
INW = 10240
C_QA, C_KA, C_VA, C_QB, C_KB, C_VB, C_GA, C_GB = 0, 1024, 1280, 1536, 3072, 4608, 6144, 8192
HALO = 1024
NE = 32
DE = 512
EPS = 1e-6
SCALE = HD ** -0.5
B_DIL = (1, 4, 16)
N_HW_SEMS = 24
N_SW_SEMS = 32
TB = 4


class Tracker:
    def __init__(self, nc, es):
        self.nc = nc
        self.eng = {'pe': nc.tensor, 'act': nc.scalar, 'dve': nc.vector, 'pool': nc.gpsimd, 'sp': nc.sync}
        self.sem = {k: es.enter_context(nc.semaphore('s_' + k)) for k in self.eng}
        self.dsem = [es.enter_context(nc.semaphore('d_%d' % i)) for i in range(N_HW_SEMS)]
        self.swsem = [es.enter_context(nc.semaphore('w_%d' % i)) for i in range(N_SW_SEMS)]
        self.swcnt = [0] * N_SW_SEMS
        self.swlast = [None] * N_SW_SEMS
        self.reset_state()

    def reset_state(self):
        self.cnt = {k: 0 for k in self.eng}
        self.known = {k: {} for k in self.eng}
        self.dcnt = [0] * N_HW_SEMS
        self.dpending = [None] * N_HW_SEMS
        self.dnext = 0
        self.swnext = 0
        self.swpending = {}
        self.lastw = {}
        self.reads = {}

    def _semof(self, key):
        if isinstance(key, str):
            return self.sem[key]
        if isinstance(key, tuple):
            return self.swsem[key[1]]
        return self.dsem[key]

    def _wait(self, e, ev):
        if ev is None:
            return
        key, val = ev
        if self.known[e].get(key, 0) >= val:
            return
        if key == 'pe' and e == 'pe':
            return
        self.eng[e].wait_ge(self._semof(key), val)
        self.known[e][key] = val
        if isinstance(key, int):
            p = self.dpending[key]
            if p is not None and p[1] <= val:
                self.dpending[key] = None
        elif isinstance(key, tuple):
            p = self.swpending.get(key)
            if p is not None and p[1] <= val:
                self.swpending.pop(key)

    def _deps(self, e, reads, writes):
        for r in reads:
            self._wait(e, self.lastw.get(r))
        for w in writes:
            self._wait(e, self.lastw.get(w))
            for ev in self.reads.get(w, ()):
                self._wait(e, ev)

    def _record(self, ev, reads, writes):
        for r in reads:
            lst = self.reads.setdefault(r, [])
            lst.append(ev)
            if len(lst) > 48:
                best = {}
                for k, v in lst:
                    best[k] = max(best.get(k, 0), v)
                self.reads[r] = list(best.items())
        for w in writes:
            self.lastw[w] = ev
            self.reads[w] = []

    cut = None
    nops = 0
    sw_clear = False

    def op(self, e, fn, reads=(), writes=()):
        Tracker.nops += 1
        if Tracker.cut is not None and Tracker.nops > Tracker.cut:
            return None
        self._deps(e, reads, writes)
        ins = fn(self.eng[e])
        self.cnt[e] += 1
        ins.then_inc(self.sem[e], 1)
        ev = (e, self.cnt[e])
        self._record(ev, reads, writes)
        return ev

    def dma(self, e, fn, reads=(), writes=()):
        Tracker.nops += 1
        if Tracker.cut is not None and Tracker.nops > Tracker.cut:
            return None
        if e == 'pool':
            if Tracker.sw_clear and self.swnext >= N_SW_SEMS:
                self.sync_all()
            self._deps(e, reads, writes)
            si = self.swnext % N_SW_SEMS
            self.swnext += 1
            if self.swlast[si] is not None:
                self._wait('pool', self.swlast[si])
            ins = fn(self.eng[e])
            self.swcnt[si] += 16
            ins.then_inc(self.swsem[si], 16)
            ev = (('w', si), self.swcnt[si])
            self.swlast[si] = ev
            self.swpending[('w', si)] = ev
        else:
            self._deps(e, reads, writes)
            i = self.dnext
            self.dnext = (self.dnext + 1) % N_HW_SEMS
            p = self.dpending[i]
            if p is not None:
                self._wait(e, (i, p[1]))
                self.dpending[i] = None
            ins = fn(self.eng[e])
            self.dcnt[i] += 16
            ins.then_inc(self.dsem[i], 16)
            ev = (i, self.dcnt[i])
            self.dpending[i] = (e, self.dcnt[i])
        self._record(ev, reads, writes)
        return ev

    def drain(self):
        for key, ev in list(self.swpending.items()):
            self._wait('pool', ev)
        for i in range(N_HW_SEMS):
            p = self.dpending[i]
            if p is not None:
                self._wait(p[0], (i, p[1]))
                self.dpending[i] = None

    def sync_all(self):
        self.drain()
        nc = self.nc
        nc.all_engine_barrier()
        for k in self.eng:
            self.eng[k].sem_clear(self.sem[k])
        for i in range(N_HW_SEMS):
            if self.dcnt[i]:
                nc.sync.sem_clear(self.dsem[i])
        if Tracker.sw_clear:
            for i in range(min(self.swnext, N_SW_SEMS)):
                nc.gpsimd.sem_clear(self.swsem[i])
            self.swcnt = [0] * N_SW_SEMS
        self.swlast = [None] * N_SW_SEMS
        nc.all_engine_barrier()
        self.reset_state()

    def maybe_sync(self, limit=24000):
        if max(self.cnt.values()) > limit or max(self.dcnt) > limit:
            self.sync_all()


def build(OWN, CAP, stages=None, dbg=()):
    SEQS = (2 * OWN, 4 * OWN)
    REST = (SEQS[0] - OWN, SEQS[1] - OWN)
    EXT = OWN + 2 * HALO
    NOWN = 2 * OWN
    NT_OWN = NOWN // 128
    XROWS = NE * CAP
    on = (lambda s: True) if stages is None else (lambda s: s in stages)

    nc = bass.Bass("TRN2", target_bir_lowering=False)

    in_names = []

    def din(name, shape, dt=F32):
        in_names.append(name)
        return nc.dram_tensor(name, list(shape), dt, kind="ExternalInput").ap()

    def dscr(name, shape, dt):
        kind = "ExternalOutput" if name in dbg else "Internal"
        return nc.dram_tensor(name, list(shape), dt, kind=kind).ap()

    xo = din("xo", [NOWN, D]); xr = din("xr", [REST[0] + REST[1], D]); xh = din("xh", [4 * HALO, D])
    tA_o = din("tA_o", [NOWN, 128]); tB_o = din("tB_o", [NOWN, 32])
    tA_r = din("tA_r", [REST[0] + REST[1], 128]); tB_h = din("tB_h", [4 * HALO, 32])
    vh = din("vh", [4 * HALO // TB, TB])
    g_mix = din("g_mix", [1, D]); g_ffn = din("g_ffn", [1, D])
    g_qa = din("g_qa", [1, HD]); g_ka = din("g_ka", [1, HD]); g_qb = din("g_qb", [1, HD]); g_kb = din("g_kb", [1, HD])
    w_in = din("w_in", [D, INW]); w_oa = din("w_oa", [1024, D]); w_ob = din("w_ob", [512, D]); w_out = din("w_out", [D, D])
    w_r = din("w_r", [D, 36]); b_r = din("b_r", [1, 36])
    if on('experts'):
        w1 = din("w1", [NE, D, DE]); w3 = din("w3", [NE, D, DE]); w2 = din("w2", [NE, DE, D])
    y = nc.dram_tensor("y", [NOWN, D], F32, kind="ExternalOutput").ap()

    hT_own = dscr("hT_own", [KC, 128, NOWN], BF16)
    qaT = dscr("qaT", [8, 128, NOWN], BF16)
    kaT = [dscr("kaT%d" % c, [2, 128, SEQS[c]], BF16) for c in range(2)]
    va = [dscr("va%d" % c, [SEQS[c], 256], BF16) for c in range(2)]
    qbT = dscr("qbT", [12, 128, NOWN], BF16)
    kbT = [dscr("kbT%d" % c, [12, 128, EXT], BF16) for c in range(2)]
    vbx = [dscr("vbx%d" % c, [EXT, 12 * 129], BF16) for c in range(2)]
    sg = dscr("sg", [NOWN, 4096], BF16)
    aoT = dscr("aoT", [8, 128, NOWN], BF16)
    bnum = dscr("bnum", [3, NOWN, 4 * 129], F32)
    x_mid = dscr("x_mid", [NOWN, D], F32)
    h2 = dscr("h2", [NOWN, D], BF16)
    xin = dscr("xin", [XROWS, D], BF16)
    eout = dscr("eout", [XROWS, D], BF16)
    dest_d = dscr("dest_d", [NT_OWN * 128, 2], I32)
    wts_d = dscr("wts_d", [NT_OWN * 128, 2], F32)

    with ExitStack() as es:
        T = Tracker(nc, es)

        def sb(es_, name, shape, dt):
            return es_.enter_context(nc.sbuf_tensor(name, list(shape), dt))

        def ps(es_, name, shape, dt):
            return es_.enter_context(nc.psum_tensor(name, list(shape), dt))

        ident_b = sb(es, "ident_b", [128, 128], BF16)
        ident_f = sb(es, "ident_f", [128, 128], F32)
        ones_b = sb(es, "ones_b", [128, 128], BF16)
        gh = sb(es, "gh", [128, 4, HD], F32)
        negC = sb(es, "negC", [128, 2], F32)
        gmx = sb(es, "gmx", [128, 4], F32)
        gh2 = sb(es, "gh2", [128, 4, HD], F32)
        epsb = sb(es, "epsb", [128, 1], F32)

        for t_, nm in ((ident_b, 'ident_b'), (ident_f, 'ident_f')):
            T.op('pool', lambda g, t_=t_: g.memset(t_[:], 1.0), writes=[nm])
            T.op('pool', lambda g, t_=t_: g.affine_select(out=t_[:], in_=t_[:], pattern=[[-1, 128]], compare_op=ALU.is_equal,
                                                          fill=0.0, base=0, channel_multiplier=1), reads=[nm], writes=[nm])
        T.op('pool', lambda g: g.memset(ones_b[:], 1.0), writes=['ones_b'])
        T.op('pool', lambda g: g.memset(epsb[:], EPS), writes=['epsb'])
        for k_, gsrc in enumerate((g_qa, g_ka, g_qb, g_kb)):
            T.dma('sp', lambda e, k_=k_, gsrc=gsrc: e.dma_start(out=gh[:, k_, :], in_=gsrc.to_broadcast([128, HD])), writes=['gh'])
        T.op('dve', lambda e: e.tensor_scalar(out=gh2[:], in0=gh[:], scalar1=-1.0, scalar2=None, op0=ALU.mult), reads=['gh'], writes=['gh2'])
        T.op('dve', lambda e: e.tensor_tensor(out=gh2[:], in0=gh2[:], in1=gh[:], op=ALU.max), reads=['gh', 'gh2'], writes=['gh2'])
        T.op('dve', lambda e: e.tensor_reduce(out=gmx[:], in_=gh2[:], axis=AX.X, op=ALU.max), reads=['gh2'], writes=['gmx'])
        T.op('dve', lambda e: e.tensor_tensor(out=negC[:, 0:1], in0=gmx[:, 0:1], in1=gmx[:, 1:2], op=ALU.mult), reads=['gmx'], writes=['negC'])
        T.op('dve', lambda e: e.tensor_tensor(out=negC[:, 1:2], in0=gmx[:, 2:3], in1=gmx[:, 3:4], op=ALU.mult), reads=['gmx', 'negC'], writes=['negC'])
        T.op('dve', lambda e: e.tensor_scalar(out=negC[:], in0=negC[:], scalar1=-math.sqrt(HD), scalar2=None, op0=ALU.mult), reads=['negC'], writes=['negC'])
        T.sync_all()

        def proj_pass(pname, ntok, wsegs, from_x, x_src, hT_src, hT_dst, make_groups):
            WC = sum(w for _, w in wsegs)
            with ExitStack() as s2:
                wt = sb(s2, pname + "_wt", [128, KC, WC], BF16)
                hst = sb(s2, pname + "_hst", [128, KC, TB * 128], BF16)
                hbufs = [hst] if from_x else [hst, sb(s2, pname + "_hst2", [128, KC, TB * 128], BF16)]
                xt = [sb(s2, pname + "_xt%d" % k, [128, D], F32) for k in range(2)] if from_x else None
                hb = [sb(s2, pname + "_hb%d" % k, [128, D], BF16) for k in range(2)] if from_x else None
                junk = sb(s2, pname + "_junk", [128, D], BF16) if from_x else None
                gmix_b = sb(s2, pname + "_gmix", [128, D], F32) if from_x else None
                if from_x:
                    T.dma('sp', lambda e: e.dma_start(out=gmix_b[:], in_=g_mix.to_broadcast([128, D])), writes=['gmix_b'])
                ss = sb(s2, pname + "_ss", [128, 2], F32) if from_x else None
                pt = ps(s2, pname + "_pt", [128, KC, 128], BF16) if from_x else None
                pj = [ps(s2, pname + "_pj%d" % k, [128, 512], F32) for k in range(3)]
                ctx = dict(s2=s2, pname=pname)
                groups, flush = make_groups(ctx)
                off = 0
                for (c0, w) in wsegs:
                    for kc in range(0, KC, 4):
                        T.dma('pool', lambda g, c0=c0, w=w, off=off, kc=kc: g.dma_start(
                            out=wt[:, kc:kc + 4, off:off + w],
                            in_=w_in[kc * 128:(kc + 4) * 128, c0:c0 + w].rearrange("(c p) n -> p c n", p=128)), writes=['wt'])
                    off += w
                T.sync_all()
                nsup = ntok // (TB * 128)

                def xpath(gt_):
                    b = gt_ % 2
                    T.dma('sp', lambda e: e.dma_start(out=xt[b][:], in_=x_src[gt_ * 128:(gt_ + 1) * 128, :]), writes=['xt%d' % b])
                    T.op('pool', lambda e: e.memset(ss[:, b:b + 1], 0.0), writes=['ss%d' % b])
                    T.op('act', lambda e: e.activation(out=junk[:], in_=xt[b][:], func=AF.Square, accum_out=ss[:, b:b + 1]),
                         reads=['xt%d' % b, 'ss%d' % b], writes=['junk', 'ss%d' % b])
                    T.op('act', lambda e: e.activation(out=ss[:, b:b + 1], in_=ss[:, b:b + 1], func=AF.Sqrt, bias=epsb[:], scale=1.0 / D),
                         reads=['ss%d' % b, 'epsb'], writes=['ss%d' % b])
                    T.op('dve', lambda e: e.reciprocal(out=ss[:, b:b + 1], in_=ss[:, b:b + 1]), reads=['ss%d' % b], writes=['ss%d' % b])
                    T.op('dve', lambda e: e.scalar_tensor_tensor(out=hb[b][:], in0=xt[b][:], scalar=ss[:, b:b + 1], in1=gmix_b[:],
                                                                  op0=ALU.mult, op1=ALU.mult),
                         reads=['xt%d' % b, 'ss%d' % b, 'gmix_b'], writes=['hb%d' % b])

                if from_x:
                    xpath(0)
                pending = None
                gi = 0
                for i in range(nsup):
                    hcur, hname = hst, 'hst'
                    if not from_x:
                        def hload(j):
                            T.dma('sp', lambda e: e.dma_start(out=hbufs[j % 2][:], in_=hT_src[:, :, bass.ts(j, TB * 128)].rearrange("c p t -> p c t")),
                                  writes=['hstL%d' % (j % 2)])
                        if i == 0:
                            hload(0)
                        if i + 1 < nsup:
                            hload(i + 1)
                        hcur, hname = hbufs[i % 2], 'hstL%d' % (i % 2)
                    for t in range(TB):
                        tsl = slice(t * 128, (t + 1) * 128)
                        if from_x:
                            gt_ = i * TB + t
                            b = gt_ % 2
                            for c in range(KC):
                                T.op('pe', lambda e, b=b, c=c: e.transpose(out=pt[:, c, :], in_=hb[b][:, c * 128:(c + 1) * 128], identity=ident_b[:]),
                                     reads=['hb%d' % b, 'ident_b'], writes=['pt%d' % (c // 8)])
                            T.op('act', lambda e, tsl=tsl: e.copy(out=hst[:, 0:8, tsl], in_=pt[:, 0:8, :]),
                                 reads=['pt0'], writes=['hst%d' % t])
                            T.op('dve', lambda e, tsl=tsl: e.tensor_copy(out=hst[:, 8:16, tsl], in_=pt[:, 8:16, :]),
                                 reads=['pt1'], writes=['hst%da' % t])
                            if gt_ + 1 < nsup * TB:
                                xpath(gt_ + 1)
                        hres = [hname, 'hst%d' % t, 'hst%da' % t, 'wt']
                        for (woff, width, post) in groups:
                            pb = gi % 3
                            gi += 1
                            for c in range(KC):
                                T.op('pe', lambda e, c=c, pb=pb, woff=woff, width=width, tsl=tsl, hcur=hcur: e.matmul(
                                    pj[pb][:, 0:width], lhsT=hcur[:, c, tsl], rhs=wt[:, c, woff:woff + width],
                                    start=(c == 0), stop=(c == KC - 1)), reads=hres, writes=['pj%d' % pb])
                            d_ = post(pj[pb], 'pj%d' % pb, t, i)
                            if pending is not None:
                                pending()
                            pending = d_
                    if pending is not None:
                        pending()
                        pending = None
                    if hT_dst is not None:
                        T.dma('sp', lambda e: e.dma_start(out=hT_dst[:, :, bass.ts(i, TB * 128)].rearrange("c p t -> p c t"), in_=hst[:]),
                              reads=['hst'] + ['hst%d' % t for t in range(TB)] + ['hst%da' % t for t in range(TB)])
                    flush(i)
                    T.maybe_sync()
                T.sync_all()

        def mk_qk(ctx, key, nh_total, gidx, rope, tab_src, dstT):
            s2 = ctx['s2']; pn = ctx['pname'] + key
            qst = sb(s2, pn + "_qst", [128, nh_total, TB * 128], BF16)
            sq = [sb(s2, pn + "_sq%d" % k, [128, 4, HD], F32) for k in range(2)]
            yv = [sb(s2, pn + "_y%d" % k, [128, 4, HD], F32) for k in range(2)]
            yb = [sb(s2, pn + "_yb%d" % k, [128, 4, HD], BF16) for k in range(2)]
            tt = [sb(s2, pn + "_tt%d" % k, [128, 4, 4, 64], F32) for k in range(2)]
            st4 = [sb(s2, pn + "_st%d" % k, [128, 4], F32) for k in range(2)]
            tw = 128 if rope == 'axial' else 32
            tab = [sb(s2, pn + "_tab%d" % k, [128, tw], F32) for k in range(2)]
            ptq = ps(s2, pn + "_ptq", [128, 4, 128], BF16)
            state = {'n': 0, 'tabt': {}}

            def post_factory(head0, nh):
                def post(pj_t, pjn, t, i):
                    k = state['n'] % 2
                    state['n'] += 1
                    R = lambda *a: [pn + '%s%d' % (x, k) for x in a]
                    tb_ = t % 2
                    if state['tabt'].get(tb_) != t:
                        state['tabt'][tb_] = t
                        T.dma('sp', lambda e: e.dma_start(out=tab[tb_][:], in_=tab_src[bass.ts(i, TB * 128), :][t * 128:(t + 1) * 128, :]),
                              writes=[pn + 'tab%d' % tb_])
                    tabn = pn + 'tab%d' % tb_
                    P3 = pj_t[:, 0:nh * HD].rearrange("p (h d) -> p h d", h=nh)
                    T.op('act', lambda e: e.activation(out=sq[k][:, 0:nh, :], in_=P3, func=AF.Square), reads=[pjn], writes=R('sq'))
                    T.op('dve', lambda e: e.tensor_reduce(out=st4[k][:, 0:nh], in_=sq[k][:, 0:nh, :], axis=AX.X, op=ALU.add), reads=R('sq'), writes=R('st'))
                    T.op('act', lambda e: e.activation(out=st4[k][:, 0:nh], in_=st4[k][:, 0:nh], func=AF.Sqrt, bias=epsb[:], scale=1.0 / HD),
                         reads=R('st') + ['epsb'], writes=R('st'))
                    T.op('dve', lambda e: e.reciprocal(out=st4[k][:, 0:nh], in_=st4[k][:, 0:nh]), reads=R('st'), writes=R('st'))
                    T.op('dve', lambda e: e.tensor_tensor(out=yv[k][:, 0:nh, :], in0=P3, in1=st4[k][:, 0:nh].unsqueeze(2).to_broadcast([128, nh, HD]), op=ALU.mult),
                         reads=[pjn] + R('st'), writes=R('y'))
                    T.op('pool', lambda e: e.tensor_tensor(out=yv[k][:, 0:nh, :], in0=yv[k][:, 0:nh, :], in1=gh[:, gidx, :].unsqueeze(1).to_broadcast([128, nh, HD]), op=ALU.mult),
                         reads=R('y') + ['gh'], writes=R('y'))
                    if rope == 'axial':
                        Y = yv[k][:, 0:nh, :].rearrange("p h (a b f) -> p h a b f", a=2, b=2)
                        YB = yb[k][:, 0:nh, :].rearrange("p h (a b f) -> p h a b f", a=2, b=2)
                        x1, x2 = Y[:, :, :, 0, :], Y[:, :, :, 1, :]
                        cosb = tab[tb_][:, 0:64].rearrange("p (a f) -> p a f", a=2).unsqueeze(1).to_broadcast([128, nh, 2, 32])
                        sinb = tab[tb_][:, 64:128].rearrange("p (a f) -> p a f", a=2).unsqueeze(1).to_broadcast([128, nh, 2, 32])
                        TT = [tt[k][:, 0:nh, j, :].rearrange("p h (a f) -> p h a f", a=2) for j in range(4)]
                        o1, o2 = YB[:, :, :, 0, :], YB[:, :, :, 1, :]
                    else:
                        Y = yv[k][:, 0:nh, :]
                        x1, x2 = Y[:, :, 0:16], Y[:, :, 16:32]
                        cosb = tab[tb_][:, 0:16].unsqueeze(1).to_broadcast([128, nh, 16])
                        sinb = tab[tb_][:, 16:32].unsqueeze(1).to_broadcast([128, nh, 16])
                        TT = [tt[k][:, 0:nh, j, 0:16] for j in range(4)]
                        o1, o2 = yb[k][:, 0:nh, 0:16], yb[k][:, 0:nh, 16:32]
                        T.op('act', lambda e: e.copy(out=yb[k][:, 0:nh, 32:128], in_=Y[:, :, 32:128]), reads=R('y'), writes=R('yb'))
                    T.op('pool', lambda e: e.tensor_tensor(out=TT[0], in0=x1, in1=cosb, op=ALU.mult), reads=R('y') + [tabn], writes=R('tta'))
                    T.op('dve', lambda e: e.tensor_tensor(out=TT[1], in0=x2, in1=sinb, op=ALU.mult), reads=R('y') + [tabn], writes=R('ttb'))
                    T.op('pool', lambda e: e.tensor_tensor(out=o1, in0=TT[0], in1=TT[1], op=ALU.subtract), reads=R('tta', 'ttb'), writes=R('yb'))
                    T.op('dve', lambda e: e.tensor_tensor(out=TT[2], in0=x2, in1=cosb, op=ALU.mult), reads=R('y') + [tabn], writes=R('ttc'))
                    T.op('pool', lambda e: e.tensor_tensor(out=TT[3], in0=x1, in1=sinb, op=ALU.mult), reads=R('y') + [tabn], writes=R('ttd'))
                    T.op('dve', lambda e: e.tensor_tensor(out=o2, in0=TT[2], in1=TT[3], op=ALU.add), reads=R('ttc', 'ttd'), writes=R('yb'))
                    def deferred():
                        for h in range(nh):
                            T.op('pe', lambda e, h=h: e.transpose(out=ptq[:, h, :], in_=yb[k][:, h, :], identity=ident_b[:]),
                                 reads=R('yb') + ['ident_b'], writes=[pn + 'ptq'])
                        T.op('act', lambda e: e.copy(out=qst[:, head0:head0 + nh, t * 128:(t + 1) * 128], in_=ptq[:, 0:nh, :]),
                             reads=[pn + 'ptq'], writes=[pn + 'qst'])
                    return deferred
                return post

            def flush(i):
                T.dma('sp', lambda e: e.dma_start(out=dstT[:, :, bass.ts(i, TB * 128)].rearrange("h d t -> d h t"), in_=qst[:]),
                      reads=[pn + 'qst'])
            return post_factory, flush

        def mk_v(ctx, key, width, dst_rows, nsub=None, valid_src=None):
            s2 = ctx['s2']; pn = ctx['pname'] + key
            if nsub is None:
                vst = sb(s2, pn + "_vst", [128, TB, width], BF16)
            else:
                vst = sb(s2, pn + "_vst", [128, TB, 12, 129], BF16)
                vld = sb(s2, pn + "_vld", [128, TB], F32)
                T.op('pool', lambda g: g.memset(vst[:], 1.0), writes=[pn + 'vst'])
            state = {'first': True}

            def post_factory(coff, w, g=None):
                def post(pj_t, pjn, t, i):
                    if nsub is None:
                        T.op('act', lambda e: e.copy(out=vst[:, t, coff:coff + w], in_=pj_t[:, 0:w]), reads=[pjn], writes=[pn + 'vst'])
                    else:
                        T.op('act', lambda e: e.copy(out=vst[:, t, g * 4:(g + 1) * 4, 0:128], in_=pj_t[:, 0:512].rearrange("p (h d) -> p h d", h=4)), reads=[pjn], writes=[pn + 'vst'])
                return post

            def flush(i):
                if nsub is not None and valid_src is not None:
                    T.dma('sp', lambda e: e.dma_start(out=vld[:], in_=valid_src[bass.ts(i, 128), :]),
                          writes=[pn + 'vld'])
                    for t in range(TB):
                        T.op('dve', lambda e, t=t: e.tensor_copy(out=vst[:, t, :, 128], in_=vld[:, t:t + 1].to_broadcast([128, 12])),
                             reads=[pn + 'vld'], writes=[pn + 'vst'])
                if nsub is None:
                    T.dma('sp', lambda e: e.dma_start(out=dst_rows[bass.ts(i, TB * 128), :].rearrange("(t p) w -> p t w", p=128), in_=vst[:]),
                          reads=[pn + 'vst'])
                else:
                    T.dma('sp', lambda e: e.dma_start(out=dst_rows[bass.ts(i, TB * 128), :].rearrange("(t p) w -> p t w", p=128),
                                                      in_=vst[:].rearrange("p t a b -> p t (a b)")), reads=[pn + 'vst'])
            return post_factory, flush

        def mk_gate(ctx, key, dst_rows):
            s2 = ctx['s2']; pn = ctx['pname'] + key
            gst = sb(s2, pn + "_gst", [128, TB, 2048], BF16)

            def post_factory(coff):
                def post(pj_t, pjn, t, i):
                    T.op('act', lambda e: e.activation(out=gst[:, t, coff:coff + 512], in_=pj_t[:, 0:512], func=AF.Sigmoid), reads=[pjn], writes=[pn + 'gst'])
                return post

            def flush(i):
                T.dma('sp', lambda e: e.dma_start(out=dst_rows[bass.ts(i, TB * 128), :].rearrange("(t p) w -> p t w", p=128), in_=gst[:]),
                      reads=[pn + 'gst'])
            return post_factory, flush

        for ch in range(2):
            tok = slice(ch * OWN, (ch + 1) * OWN)
            if on('own1'):
                def mg(ctx, ch=ch, tok=tok):
                    pq, fq = mk_qk(ctx, 'q', 8, 0, 'axial', tA_o[tok, :], qaT[:, :, tok])
                    pk, fk = mk_qk(ctx, 'k', 2, 1, 'axial', tA_o[tok, :], kaT[ch][:, :, 0:OWN])
                    pv, fv = mk_v(ctx, 'v', 256, va[ch][0:OWN, :])
                    groups = [(0, 512, pq(0, 4)), (512, 512, pq(4, 4)), (1024, 256, pk(0, 2)), (1280, 256, pv(0, 256))]
                    return groups, (lambda i: (fq(i), fk(i), fv(i)))
                proj_pass("o1c%d" % ch, OWN, [(C_QA, 1536)], True, xo[tok, :], None, hT_own[:, :, tok], mg)
            if on('own1b'):
                def mg(ctx, ch=ch, tok=tok):
                    pv, fv = mk_v(ctx, 'v', 1536, vbx[ch][HALO:HALO + OWN, :], nsub=True)
                    return [(g * 512, 512, pv(0, 512, g)) for g in range(3)], fv
                proj_pass("o1b%d" % ch, OWN, [(C_VB, 1536)], False, None, hT_own[:, :, tok], None, mg)
            if on('own2'):
                def mg(ctx, ch=ch, tok=tok):
                    pq, fq = mk_qk(ctx, 'q', 12, 2, 'partial', tB_o[tok, :], qbT[:, :, tok])
                    return [(g * 512, 512, pq(g * 4, 4)) for g in range(3)], fq
                proj_pass("o2a%d" % ch, OWN, [(C_QB, 1536)], False, None, hT_own[:, :, tok], None, mg)

                def mg(ctx, ch=ch, tok=tok):
                    pk, fk = mk_qk(ctx, 'k', 12, 3, 'partial', tB_o[tok, :], kbT[ch][:, :, HALO:HALO + OWN])
                    return [(g * 512, 512, pk(g * 4, 4)) for g in range(3)], fk
                proj_pass("o2b%d" % ch, OWN, [(C_KB, 1536)], False, None, hT_own[:, :, tok], None, mg)
            if on('own3'):
                for gi_, c0 in enumerate((C_GA, C_GB)):
                    def mg(ctx, ch=ch, tok=tok, gi_=gi_):
                        pg, fg = mk_gate(ctx, 'g', sg[tok, gi_ * 2048:(gi_ + 1) * 2048])
                        return [(j * 512, 512, pg(j * 512)) for j in range(4)], fg
                    proj_pass("o3%d%d" % (gi_, ch), OWN, [(c0, 2048)], False, None, hT_own[:, :, tok], None, mg)
            if on('rest'):
                roff = 0 if ch == 0 else REST[0]
                rtok = slice(roff, roff + REST[ch])

                def mg(ctx, ch=ch, rtok=rtok):
                    pk, fk = mk_qk(ctx, 'k', 2, 1, 'axial', tA_r[rtok, :], kaT[ch][:, :, OWN:SEQS[ch]])
                    pv, fv = mk_v(ctx, 'v', 256, va[ch][OWN:SEQS[ch], :])
                    return [(0, 256, pk(0, 2)), (256, 256, pv(0, 256))], (lambda i: (fk(i), fv(i)))
                proj_pass("rs%d" % ch, REST[ch], [(C_KA, 512)], True, xr[rtok, :], None, None, mg)
            if on('halo'):
                for side in range(2):
                    hs = slice((ch * 2 + side) * HALO, (ch * 2 + side + 1) * HALO)
                    eo = 0 if side == 0 else HALO + OWN

                    def mg(ctx, ch=ch, hs=hs, eo=eo):
                        pk, fk = mk_qk(ctx, 'k', 12, 3, 'partial', tB_h[hs, :], kbT[ch][:, :, eo:eo + HALO])
                        return [(g * 512, 512, pk(g * 4, 4)) for g in range(3)], fk
                    proj_pass("hk%d%d" % (ch, side), HALO, [(C_KB, 1536)], True, xh[hs, :], None, None, mg)

                    def mg(ctx, ch=ch, hs=hs, eo=eo):
                        pv, fv = mk_v(ctx, 'v', 1536, vbx[ch][eo:eo + HALO, :], nsub=True, valid_src=vh[(ch * 2 + side) * (HALO // TB):(ch * 2 + side + 1) * (HALO // TB), :])
                        return [(g * 512, 512, pv(0, 512, g)) for g in range(3)], fv
                    proj_pass("hv%d%d" % (ch, side), HALO, [(C_VB, 1536)], True, xh[hs, :], None, None, mg)

        if on('attnA'):
            for ch in range(2):
                S = SEQS[ch]
                NKT = S // 128
                with ExitStack() as s2:
                    kt_sb = sb(s2, "A_kT%d" % ch, [128, 2, S], BF16)
                    v_sb = sb(s2, "A_v%d" % ch, [128, NKT, 256], BF16)
                    q_sb = sb(s2, "A_q%d" % ch, [128, 8, 128], BF16)
                    pT = [sb(s2, "A_pT%d%d" % (ch, k), [128, 512], BF16) for k in range(3)]
                    rd = sb(s2, "A_rd%d" % ch, [128, 512], F32)
                    ao = sb(s2, "A_ao%d" % ch, [128, 8, 128], BF16)
                    ps_s = [ps(s2, "A_pss%d%d" % (ch, k), [128, 512], F32) for k in range(3)]
                    ps_o = [ps(s2, "A_pso%d%d" % (ch, k), [128, 512], F32) for k in range(2)]
                    ps_d = [ps(s2, "A_psd%d%d" % (ch, k), [128, 512], F32) for k in range(2)]
                    for kv in range(2):
                        for c0 in range(0, S, min(S, 4096)):
                            T.dma('sp', lambda e, kv=kv, c0=c0: e.dma_start(out=kt_sb[:, kv, c0:c0 + min(S, 4096)], in_=kaT[ch][kv, :, c0:c0 + min(S, 4096)]), writes=['A_kT'])
                    VS = min(NKT, 32)
                    for c0 in range(0, NKT, VS):
                        T.dma('sp', lambda e, c0=c0: e.dma_start(out=v_sb[:, c0:c0 + VS, :],
                                                                 in_=va[ch][c0 * 128:(c0 + VS) * 128, :].rearrange("(t p) w -> p t w", p=128)), writes=['A_v'])
                    T.sync_all()
                    tok0 = ch * OWN
                    for i in range(OWN // 128):
                        T.dma('sp', lambda e: e.dma_start(out=q_sb[:], in_=qaT[:, :, tok0:tok0 + OWN][:, :, bass.ts(i, 128)].rearrange("h d t -> d h t")),
                              writes=['A_q'])
                        for kv in range(2):
                            q4 = q_sb[:, kv * 4:(kv + 1) * 4, :].rearrange("p h t -> p (h t)")
                            po, pd = ps_o[kv], ps_d[kv]

                            def s_mm(kt, kv=kv, q4=q4):
                                b = kt % 3
                                T.op('pe', lambda e: e.matmul(ps_s[b][:], lhsT=kt_sb[:, kv, kt * 128:(kt + 1) * 128], rhs=q4, start=True, stop=True),
                                     reads=['A_kT', 'A_q'], writes=['A_pss%d' % b])

                            def rest_(kt, kv=kv, po=po, pd=pd):
                                b = kt % 3
                                T.op('act', lambda e: e.activation(out=pT[b][:], in_=ps_s[b][:], func=AF.Exp, bias=negC[:, 0:1], scale=SCALE),
                                     reads=['A_pss%d' % b, 'negC'], writes=['A_pT%d' % b])
                                T.op('pe', lambda e: e.matmul(po[:], lhsT=v_sb[:, kt, kv * 128:(kv + 1) * 128], rhs=pT[b][:], start=(kt == 0), stop=(kt == NKT - 1)),
                                     reads=['A_v', 'A_pT%d' % b], writes=['A_pso%d' % kv])
                                T.op('pe', lambda e: e.matmul(pd[:], lhsT=ones_b[:], rhs=pT[b][:], start=(kt == 0), stop=(kt == NKT - 1)),
                                     reads=['ones_b', 'A_pT%d' % b], writes=['A_psd%d' % kv])
                            s_mm(0); s_mm(1)
                            for kt in range(NKT):
                                if kt + 2 < NKT:
                                    s_mm(kt + 2)
                                rest_(kt)
                            T.op('dve', lambda e, pd=pd: e.reciprocal(out=rd[:], in_=pd[:]), reads=['A_psd%d' % kv], writes=['A_rd'])
                            T.op('dve', lambda e, po=po, kv=kv: e.tensor_tensor(out=ao[:, kv * 4:(kv + 1) * 4, :].rearrange("p h t -> p (h t)"), in0=po[:], in1=rd[:], op=ALU.mult),
                                 reads=['A_pso%d' % kv, 'A_rd'], writes=['A_ao%d' % kv])
                        T.dma('sp', lambda e: e.dma_start(out=aoT[:, :, tok0:tok0 + OWN][:, :, bass.ts(i, 128)].rearrange("h d t -> d h t"), in_=ao[:]),
                              reads=['A_ao0', 'A_ao1'])
                        T.maybe_sync()
                    T.sync_all()

        if on('attnB'):
            with ExitStack() as s2:
                qg = sb(s2, "B_q", [128, 4, OWN], BF16)
                kg = sb(s2, "B_k", [128, 4, EXT], BF16)
                mA = sb(s2, "B_mA", [128, 4, 128], BF16)
                mB = sb(s2, "B_mB", [128, 4, 128], BF16)
                v1 = [sb(s2, "B_v%d" % k, [128, 4, 129], BF16) for k in range(4)]
                pA = [sb(s2, "B_pA%d" % k, [128, 4, 128], BF16) for k in range(2)]
                pB = [sb(s2, "B_pB%d" % k, [128, 4, 128], BF16) for k in range(2)]
                ob = [sb(s2, "B_ob%d" % k, [128, 4, 129], F32) for k in range(2)]
                psA = [ps(s2, "B_psA%d" % k, [128, 4, 128], F32) for k in range(2)]
                psB = [ps(s2, "B_psB%d" % k, [128, 4, 128], F32) for k in range(2)]
                po_ = [ps(s2, "B_po%d" % k, [128, 4, 256], F32) for k in range(1)]
                for m_, sgn in ((mA, 1), (mB, -1)):
                    nm = 'B_mA' if m_ is mA else 'B_mB'
                    T.op('pool', lambda g, m_=m_: g.memset(m_[:], 1.0), writes=[nm])
                    T.op('pool', lambda g, m_=m_, sgn=sgn: g.affine_select(out=m_[:], in_=m_[:], pattern=[[0, 4], [-sgn, 128]], compare_op=ALU.is_ge,
                                                                           fill=0.0, base=0, channel_multiplier=sgn), reads=[nm], writes=[nm])
                for ch in range(2):
                    tok0 = ch * OWN
                    for g in range(3):
                        r = B_DIL[g]
                        T.dma('sp', lambda e: e.dma_start(out=qg[:], in_=qbT[g * 4:(g + 1) * 4, :, tok0:tok0 + OWN].rearrange("h d t -> d h t")), writes=['B_q'])
                        T.dma('sp', lambda e: e.dma_start(out=kg[:], in_=kbT[ch][g * 4:(g + 1) * 4, :, :].rearrange("h d t -> d h t")), writes=['B_k'])
                        nqb = OWN // r // 128
                        vcnt = 0
                        it = 0
                        for c in range(r):
                            vt = {}
                            for qb in range(nqb):
                                u0 = qb * 128
                                for s_ in (qb, qb + 1):
                                    if s_ not in vt:
                                        k_ = vcnt % 4
                                        vcnt += 1
                                        vt[s_] = k_
                                        e0 = HALO + (128 * s_ - 64) * r + c
                                        T.dma('sp', lambda e, k_=k_, e0=e0: e.dma_start(
                                            out=v1[k_][:].rearrange("p h d -> p (h d)"),
                                            in_=vbx[ch][e0:e0 + 127 * r + 1:r, g * 516:(g + 1) * 516]), writes=['B_v%d' % k_])
                                kA, kB = vt[qb], vt[qb + 1]
                                b = it % 2
                                it += 1
                                qs = u0 * r + c
                                eA = HALO + (u0 - 64) * r + c
                                eB = HALO + (u0 + 64) * r + c
                                for h in range(4):
                                    T.op('pe', lambda e, h=h: e.matmul(psA[b][:, h, :], lhsT=kg[:, h, eA:eA + 127 * r + 1:r], rhs=qg[:, h, qs:qs + 127 * r + 1:r], start=True, stop=True),
                                         reads=['B_k', 'B_q'], writes=['B_psA%d' % b])
                                    T.op('pe', lambda e, h=h: e.matmul(psB[b][:, h, :], lhsT=kg[:, h, eB:eB + 127 * r + 1:r], rhs=qg[:, h, qs:qs + 127 * r + 1:r], start=True, stop=True),
                                         reads=['B_k', 'B_q'], writes=['B_psB%d' % b])
                                T.op('act', lambda e: e.activation(out=pA[b][:], in_=psA[b][:], func=AF.Exp, bias=negC[:, 1:2], scale=SCALE), reads=['B_psA%d' % b, 'negC'], writes=['B_pA%d' % b])
                                T.op('act', lambda e: e.activation(out=pB[b][:], in_=psB[b][:], func=AF.Exp, bias=negC[:, 1:2], scale=SCALE), reads=['B_psB%d' % b, 'negC'], writes=['B_pB%d' % b])
                                T.op('dve', lambda e: e.tensor_tensor(out=pA[b][:], in0=pA[b][:], in1=mA[:], op=ALU.mult), reads=['B_pA%d' % b, 'B_mA'], writes=['B_pA%d' % b])
                                T.op('pool', lambda e: e.tensor_tensor(out=pB[b][:], in0=pB[b][:], in1=mB[:], op=ALU.mult), reads=['B_pB%d' % b, 'B_mB'], writes=['B_pB%d' % b])
                                for h in range(4):
                                    T.op('pe', lambda e, h=h: e.matmul(po_[0][:, h, 0:129], lhsT=pA[b][:, h, :], rhs=v1[kA][:, h, :], start=True, stop=False),
                                         reads=['B_pA%d' % b, 'B_v%d' % kA], writes=['B_po'])
                                    T.op('pe', lambda e, h=h: e.matmul(po_[0][:, h, 0:129], lhsT=pB[b][:, h, :], rhs=v1[kB][:, h, :], start=False, stop=True),
                                         reads=['B_pB%d' % b, 'B_v%d' % kB], writes=['B_po'])
                                T.op('act', lambda e: e.copy(out=ob[b][:], in_=po_[0][:, :, 0:129]), reads=['B_po'], writes=['B_ob%d' % b])
                                t0 = tok0 + qs
                                T.dma('sp', lambda e: e.dma_start(out=bnum[g, t0:t0 + 127 * r + 1:r, :], in_=ob[b][:].rearrange("p h d -> p (h d)")), reads=['B_ob%d' % b])
                        T.sync_all()

        bc_reg = nc.gpsimd.alloc_register("bc")
        nc.gpsimd.reg_mov(bc_reg, XROWS - 1)
        if on('mix'):
            with ExitStack() as s2:
                woa = sb(s2, "M_woa", [128, 8, D], BF16)
                wob = sb(s2, "M_wob", [128, 4, D], BF16)
                wo = sb(s2, "M_wo", [128, KC, D], BF16)
                wr = sb(s2, "M_wr", [128, KC, 36], F32)
                br = sb(s2, "M_br", [128, 36], F32)
                ecap = sb(s2, "M_ecap", [128, NE], F32)
                base = sb(s2, "M_base", [128, NE], F32)
                utri = sb(s2, "M_utri", [128, 128], BF16)
                aot = sb(s2, "M_aot", [128, 8, 128], BF16)
                bn = sb(s2, "M_bn", [128, 3, 516], F32)
                bs = sb(s2, "M_bs", [128, 4, 129], F32)
                rdn = sb(s2, "M_rdn", [128, 4], F32)
                bo = sb(s2, "M_bo", [128, 4, 128], BF16)
                boT = sb(s2, "M_boT", [128, 4, 128], BF16)
                sgt = sb(s2, "M_sg", [128, 4096], BF16)
                xt = sb(s2, "M_xt", [128, D], F32)
                m1 = sb(s2, "M_m1", [128, 512], F32)
                m2 = sb(s2, "M_m2", [128, 512], F32)
                mixed = sb(s2, "M_mixed", [128, D], BF16)
                mixT = sb(s2, "M_mixT", [128, KC, 128], BF16)
                xm = sb(s2, "M_xm", [128, D], F32)
                gffn_b = sb(s2, "M_gffn", [128, D], F32)
                T.dma('sp', lambda e: e.dma_start(out=gffn_b[:], in_=g_ffn.to_broadcast([128, D])), writes=['gffn_b'])
                ssq = sb(s2, "M_ssq", [128, 1], F32)
                h2f = sb(s2, "M_h2f", [128, D], F32)
                h2b = sb(s2, "M_h2b", [128, D], BF16)
                h2T = sb(s2, "M_h2T", [128, 8, 128], F32)
                lg = sb(s2, "M_lg", [128, 36], F32)
                sm = sb(s2, "M_sm", [128, 16], F32)
                ohg = sb(s2, "M_ohg", [128, 4], F32)
                les = sb(s2, "M_les", [128, 4, 8], F32)
                lsel = sb(s2, "M_lsel", [128, 8], F32)
                oh1 = sb(s2, "M_oh1", [128, 8], F32)
                oh2 = sb(s2, "M_oh2", [128, 8], F32)
                msk = sb(s2, "M_msk", [128, 8], F32)
                o32 = sb(s2, "M_o32", [128, 2, 4, 8], F32)
                Mb = sb(s2, "M_Mb", [128, NE], BF16)
                tot = sb(s2, "M_tot", [128, NE], F32)
                rk = sb(s2, "M_rk", [128, NE], F32)
                tmp32 = sb(s2, "M_tmp32", [128, NE], F32)
                dst_f = sb(s2, "M_dstf", [128, 2], F32)
                rk_f = sb(s2, "M_rkf", [128, 2], F32)
                dst_i = sb(s2, "M_dsti", [128, 2], I32)
                wts = sb(s2, "M_wts", [128, 2], F32)
                pj = [ps(s2, "M_pj%d" % k, [128, 512], F32) for k in range(3)]
                ptb = ps(s2, "M_ptb", [128, KC, 128], BF16)
                ptf = ps(s2, "M_ptf", [128, 8, 128], F32)
                plog = ps(s2, "M_plog", [128, 64], F32)
                for kc in range(0, 8, 4):
                    T.dma('pool', lambda g, kc=kc: g.dma_start(out=woa[:, kc:kc + 4, :], in_=w_oa[kc * 128:(kc + 4) * 128, :].rearrange("(c p) n -> p c n", p=128)), writes=['M_w'])
                T.dma('pool', lambda g: g.dma_start(out=wob[:], in_=w_ob.rearrange("(c p) n -> p c n", p=128)), writes=['M_w'])
                for kc in range(0, KC, 4):
                    T.dma('pool', lambda g, kc=kc: g.dma_start(out=wo[:, kc:kc + 4, :], in_=w_out[kc * 128:(kc + 4) * 128, :].rearrange("(c p) n -> p c n", p=128)), writes=['M_w'])
                T.dma('sp', lambda e: e.dma_start(out=wr[:], in_=w_r.rearrange("(c p) n -> p c n", p=128)), writes=['M_w'])
                T.dma('sp', lambda e: e.dma_start(out=br[:], in_=b_r.to_broadcast([128, 36])), writes=['M_w'])
                T.op('pool', lambda g: g.iota(ecap[:], pattern=[[CAP, NE]], base=0, channel_multiplier=0, allow_small_or_imprecise_dtypes=True), writes=['M_w'])
                T.op('pool', lambda g: g.memset(base[:], 0.0), writes=['M_base'])
                T.op('pool', lambda g: g.memset(h2f[:], 0.0), writes=['h2f'])
                h2fz = h2f[:].bitcast(BF16)
                for r0 in range(0, XROWS, 256):
                    T.dma('sp', lambda e, r0=r0: e.dma_start(out=xin[r0:r0 + 256, :].rearrange("(p a) w -> p (a w)", p=128), in_=h2fz), reads=['h2f'], writes=['xin'])
                T.op('pool', lambda g: g.memset(utri[:], 1.0), writes=['M_utri'])
                T.op('pool', lambda g: g.affine_select(out=utri[:], in_=utri[:], pattern=[[1, 128]], compare_op=ALU.is_ge, fill=0.0, base=-1, channel_multiplier=-1),
                     reads=['M_utri'], writes=['M_utri'])
                T.sync_all()
                for i in range(NT_OWN):
                    rows = bass.ts(i, 128)
                    T.dma('sp', lambda e: e.dma_start(out=aot[:], in_=aoT[:, :, rows].rearrange("h d t -> d h t")), writes=['aot'])
                    T.dma('sp', lambda e: e.dma_start(out=bn[:], in_=bnum[:, rows, :].rearrange("g t w -> t g w")), writes=['bn'])
                    T.dma('sp', lambda e: e.dma_start(out=sgt[:], in_=sg[rows, :]), writes=['sgt'])
                    T.dma('sp', lambda e: e.dma_start(out=xt[:], in_=xo[rows, :]), writes=['xt'])
                    bs2 = bs[:].rearrange("p h d -> p (h d)")
                    T.op('dve', lambda e: e.tensor_tensor(out=bs2, in0=bn[:, 0, :], in1=bn[:, 1, :], op=ALU.add), reads=['bn'], writes=['bs'])
                    T.op('dve', lambda e: e.tensor_tensor(out=bs2, in0=bs2, in1=bn[:, 2, :], op=ALU.add), reads=['bn', 'bs'], writes=['bs'])
                    T.op('dve', lambda e: e.reciprocal(out=rdn[:], in_=bs[:, :, 128]), reads=['bs'], writes=['rdn'])
                    T.op('dve', lambda e: e.tensor_tensor(out=bo[:], in0=bs[:, :, 0:128], in1=rdn[:].unsqueeze(2).to_broadcast([128, 4, 128]), op=ALU.mult),
                         reads=['bs', 'rdn'], writes=['bo'])
                    for h in range(4):
                        T.op('pe', lambda e, h=h: e.transpose(out=ptb[:, h, :], in_=bo[:, h, :], identity=ident_b[:]), reads=['bo', 'ident_b'], writes=['ptb'])
                    T.op('act', lambda e: e.copy(out=boT[:], in_=ptb[:, 0:4, :]), reads=['ptb'], writes=['boT'])
                    for j in range(4):
                        cs = slice(j * 512, (j + 1) * 512)
                        pa, pb_ = pj[0], pj[1]
                        for h in range(8):
                            T.op('pe', lambda e, h=h: e.matmul(pa[:], lhsT=aot[:, h, :], rhs=woa[:, h, cs], start=(h == 0), stop=(h == 7)), reads=['aot', 'M_w'], writes=['pj0'])
                        for h in range(4):
                            T.op('pe', lambda e, h=h: e.matmul(pb_[:], lhsT=boT[:, h, :], rhs=wob[:, h, cs], start=(h == 0), stop=(h == 3)), reads=['boT', 'M_w'], writes=['pj1'])
                        T.op('dve', lambda e: e.tensor_tensor(out=m1[:], in0=pa[:], in1=sgt[:, j * 512:(j + 1) * 512], op=ALU.mult), reads=['pj0', 'sgt'], writes=['m1'])
                        T.op('dve', lambda e: e.tensor_tensor(out=m2[:], in0=pb_[:], in1=sgt[:, 2048 + j * 512:2048 + (j + 1) * 512], op=ALU.mult), reads=['pj1', 'sgt'], writes=['m2'])
                        T.op('pool', lambda e: e.tensor_tensor(out=mixed[:, cs], in0=m1[:], in1=m2[:], op=ALU.add), reads=['m1', 'm2'], writes=['mixed'])
                    for c in range(KC):
                        T.op('pe', lambda e, c=c: e.transpose(out=ptb[:, c, :], in_=mixed[:, c * 128:(c + 1) * 128], identity=ident_b[:]), reads=['mixed', 'ident_b'], writes=['ptb'])
                    T.op('act', lambda e: e.copy(out=mixT[:, 0:8, :], in_=ptb[:, 0:8, :]), reads=['ptb'], writes=['mixTa'])
                    T.op('dve', lambda e: e.tensor_copy(out=mixT[:, 8:16, :], in_=ptb[:, 8:16, :]), reads=['ptb'], writes=['mixTb'])
                    for j in range(4):
                        cs = slice(j * 512, (j + 1) * 512)
                        pbk = pj[2] if j % 2 == 0 else pj[0]
                        pbn = 'pj2' if j % 2 == 0 else 'pj0'
                        for c in range(KC):
                            T.op('pe', lambda e, c=c: e.matmul(pbk[:], lhsT=mixT[:, c, :], rhs=wo[:, c, cs], start=(c == 0), stop=(c == KC - 1)), reads=['mixTa', 'mixTb', 'M_w'], writes=[pbn])
                        T.op('dve', lambda e: e.tensor_tensor(out=xm[:, cs], in0=pbk[:], in1=xt[:, cs], op=ALU.add), reads=[pbn, 'xt'], writes=['xm'])
                    T.dma('sp', lambda e: e.dma_start(out=x_mid[rows, :], in_=xm[:]), reads=['xm'])
                    T.op('pool', lambda e: e.memset(ssq[:], 0.0), writes=['ssq'])
                    T.op('act', lambda e: e.activation(out=mixed[:], in_=xm[:], func=AF.Square, accum_out=ssq[:]), reads=['xm', 'ssq'], writes=['mixed', 'ssq'])
                    T.op('act', lambda e: e.activation(out=ssq[:], in_=ssq[:], func=AF.Sqrt, bias=epsb[:], scale=1.0 / D), reads=['ssq', 'epsb'], writes=['ssq'])
                    T.op('dve', lambda e: e.reciprocal(out=ssq[:], in_=ssq[:]), reads=['ssq'], writes=['ssq'])
                    T.op('dve', lambda e: e.scalar_tensor_tensor(out=h2f[:], in0=xm[:], scalar=ssq[:], in1=gffn_b[:], op0=ALU.mult, op1=ALU.mult), reads=['xm', 'ssq', 'gffn_b'], writes=['h2f'])
                    T.op('act', lambda e: e.copy(out=h2b[:], in_=h2f[:]), reads=['h2f'], writes=['h2b'])
                    T.dma('sp', lambda e: e.dma_start(out=h2[rows, :], in_=h2b[:]), reads=['h2b'])
                    for half in range(2):
                        for c in range(8):
                            cc = half * 8 + c
                            T.op('pe', lambda e, c=c, cc=cc: e.transpose(out=ptf[:, c, :], in_=h2f[:, cc * 128:(cc + 1) * 128], identity=ident_f[:]), reads=['h2f', 'ident_f'], writes=['ptf'])
                        T.op('act', lambda e: e.copy(out=h2T[:, 0:4, :], in_=ptf[:, 0:4, :]), reads=['ptf'], writes=['h2Ta'])
                        T.op('dve', lambda e: e.tensor_copy(out=h2T[:, 4:8, :], in_=ptf[:, 4:8, :]), reads=['ptf'], writes=['h2Tb'])
                        for c in range(8):
                            cc = half * 8 + c
                            T.op('pe', lambda e, c=c, cc=cc: e.matmul(plog[:, 0:36], lhsT=h2T[:, c, :], rhs=wr[:, cc, :], start=(cc == 0), stop=(cc == KC - 1)),
                                 reads=['h2Ta', 'h2Tb', 'M_w'], writes=['plog'])
                    T.op('dve', lambda e: e.tensor_tensor(out=lg[:], in0=plog[:, 0:36], in1=br[:], op=ALU.add), reads=['plog', 'M_w'], writes=['lg'])
                    V = lambda e: e
                    T.op('dve', lambda e: e.tensor_reduce(out=sm[:, 0:1], in_=lg[:, 0:4], axis=AX.X, op=ALU.max), reads=['lg'], writes=['sm0'])
                    T.op('dve', lambda e: e.tensor_scalar(out=ohg[:], in0=lg[:, 0:4], scalar1=sm[:, 0:1], scalar2=None, op0=ALU.is_equal), reads=['lg', 'sm0'], writes=['ohg'])
                    T.op('dve', lambda e: e.tensor_scalar(out=sm[:, 1:2], in0=sm[:, 0:1], scalar1=-1.0, scalar2=None, op0=ALU.mult), reads=['sm0'], writes=['sm1'])
                    T.op('pool', lambda e: e.memset(sm[:, 2:3], 0.0), writes=['sm2'])
                    T.op('act', lambda e: e.activation(out=sm[:, 4:8], in_=lg[:, 0:4], func=AF.Exp, bias=sm[:, 1:2], scale=1.0, accum_out=sm[:, 2:3]), reads=['lg', 'sm1', 'sm2'], writes=['sm2', 'sm4'])
                    T.op('dve', lambda e: e.reciprocal(out=sm[:, 3:4], in_=sm[:, 2:3]), reads=['sm2'], writes=['sm3'])
                    T.op('dve', lambda e: e.tensor_tensor(out=les[:], in0=lg[:, 4:36].rearrange("p (g x) -> p g x", g=4), in1=ohg[:].unsqueeze(2).to_broadcast([128, 4, 8]), op=ALU.mult),
                         reads=['lg', 'ohg'], writes=['les'])
                    T.op('dve', lambda e: e.tensor_reduce(out=lsel[:], in_=les[:].rearrange("p g x -> p x g"), axis=AX.X, op=ALU.add), reads=['les'], writes=['lsel'])
                    T.op('dve', lambda e: e.tensor_reduce(out=sm[:, 8:9], in_=lsel[:], axis=AX.X, op=ALU.max), reads=['lsel'], writes=['sm8'])
                    T.op('dve', lambda e: e.tensor_scalar(out=oh1[:], in0=lsel[:], scalar1=sm[:, 8:9], scalar2=None, op0=ALU.is_equal), reads=['lsel', 'sm8'], writes=['oh1'])
                    T.op('dve', lambda e: e.scalar_tensor_tensor(out=msk[:], in0=oh1[:], scalar=-1e30, in1=lsel[:], op0=ALU.mult, op1=ALU.add), reads=['oh1', 'lsel'], writes=['msk'])
                    T.op('dve', lambda e: e.tensor_reduce(out=sm[:, 9:10], in_=msk[:], axis=AX.X, op=ALU.max), reads=['msk'], writes=['sm9'])
                    T.op('dve', lambda e: e.tensor_scalar(out=oh2[:], in0=msk[:], scalar1=sm[:, 9:10], scalar2=None, op0=ALU.is_equal), reads=['msk', 'sm9'], writes=['oh2'])
                    T.op('dve', lambda e: e.tensor_tensor(out=sm[:, 10:11], in0=sm[:, 9:10], in1=sm[:, 8:9], op=ALU.subtract), reads=['sm8', 'sm9'], writes=['sm10'])
                    T.op('act', lambda e: e.activation(out=sm[:, 11:12], in_=sm[:, 10:11], func=AF.Exp), reads=['sm10'], writes=['sm11'])
                    T.op('dve', lambda e: e.tensor_scalar(out=sm[:, 12:13], in0=sm[:, 11:12], scalar1=1.0, scalar2=None, op0=ALU.add), reads=['sm11'], writes=['sm12'])
                    T.op('dve', lambda e: e.reciprocal(out=sm[:, 13:14], in_=sm[:, 12:13]), reads=['sm12'], writes=['sm13'])
                    T.op('dve', lambda e: e.tensor_tensor(out=wts[:, 0:1], in0=sm[:, 13:14], in1=sm[:, 3:4], op=ALU.mult), reads=['sm13', 'sm3'], writes=['wts'])
                    T.op('dve', lambda e: e.tensor_tensor(out=wts[:, 1:2], in0=wts[:, 0:1], in1=sm[:, 11:12], op=ALU.mult), reads=['wts', 'sm11'], writes=['wts'])
                    for k_, ohk in enumerate((oh1, oh2)):
                        T.op('dve', lambda e, k_=k_, ohk=ohk: e.tensor_tensor(out=o32[:, k_, :, :], in0=ohg[:].unsqueeze(2).to_broadcast([128, 4, 8]),
                                                                              in1=ohk[:].unsqueeze(1).to_broadcast([128, 4, 8]), op=ALU.mult), reads=['ohg', 'oh1', 'oh2'], writes=['o32_%d' % k_])
                    o32f = o32[:].rearrange("p k g x -> p k (g x)")
                    T.op('dve', lambda e: e.tensor_tensor(out=Mb[:], in0=o32f[:, 0, :], in1=o32f[:, 1, :], op=ALU.add), reads=['o32_0', 'o32_1'], writes=['Mb'])
                    T.op('pe', lambda e: e.matmul(plog[:, 0:32], lhsT=utri[:], rhs=Mb[:], start=True, stop=True), reads=['Mb', 'M_utri', 'lg'], writes=['plog'])
                    T.op('dve', lambda e: e.tensor_tensor(out=rk[:], in0=plog[:, 0:32], in1=base[:], op=ALU.add), reads=['plog', 'M_base'], writes=['rk'])
                    T.op('pe', lambda e: e.matmul(plog[:, 32:64], lhsT=ones_b[:], rhs=Mb[:], start=True, stop=True), reads=['Mb', 'ones_b'], writes=['plog'])
                    T.op('dve', lambda e: e.tensor_tensor(out=base[:], in0=plog[:, 32:64], in1=base[:], op=ALU.add), reads=['plog', 'M_base', 'rk'], writes=['M_base'])
                    T.op('dve', lambda e: e.tensor_tensor(out=tot[:], in0=rk[:], in1=ecap[:], op=ALU.add), reads=['rk', 'M_w'], writes=['tot'])
                    for k_ in range(2):
                        T.op('dve', lambda e, k_=k_: e.tensor_tensor(out=tmp32[:], in0=o32f[:, k_, :], in1=tot[:], op=ALU.mult), reads=['o32_%d' % k_, 'tot'], writes=['tmp32'])
                        T.op('dve', lambda e, k_=k_: e.tensor_reduce(out=dst_f[:, k_:k_ + 1], in_=tmp32[:], axis=AX.X, op=ALU.add), reads=['tmp32'], writes=['dstf'])
                        T.op('dve', lambda e, k_=k_: e.tensor_tensor(out=tmp32[:], in0=o32f[:, k_, :], in1=rk[:], op=ALU.mult), reads=['o32_%d' % k_, 'rk', 'dstf'], writes=['tmp32'])
                        T.op('dve', lambda e, k_=k_: e.tensor_reduce(out=rk_f[:, k_:k_ + 1], in_=tmp32[:], axis=AX.X, op=ALU.add), reads=['tmp32'], writes=['rkf'])
                    T.op('dve', lambda e: e.tensor_scalar(out=rk_f[:], in0=rk_f[:], scalar1=float(CAP), scalar2=None, op0=ALU.is_ge), reads=['rkf'], writes=['rkf'])
                    T.op('dve', lambda e: e.scalar_tensor_tensor(out=dst_f[:], in0=rk_f[:], scalar=1.0e6, in1=dst_f[:], op0=ALU.mult, op1=ALU.add), reads=['rkf', 'dstf'], writes=['dstf'])
                    T.op('dve', lambda e: e.tensor_scalar(out=rk_f[:], in0=rk_f[:], scalar1=-1.0, scalar2=1.0, op0=ALU.mult, op1=ALU.add), reads=['rkf', 'dstf'], writes=['rkf'])
                    T.op('dve', lambda e: e.tensor_tensor(out=wts[:], in0=wts[:], in1=rk_f[:], op=ALU.mult), reads=['rkf', 'wts'], writes=['wts'])
                    T.op('dve', lambda e: e.tensor_copy(out=dst_i[:], in_=dst_f[:]), reads=['dstf'], writes=['dsti'])
                    T.dma('sp', lambda e: e.dma_start(out=dest_d[rows, :], in_=dst_i[:]), reads=['dsti'])
                    T.dma('sp', lambda e: e.dma_start(out=wts_d[rows, :], in_=wts[:]), reads=['wts'])
                    for k_ in range(2):
                        T.dma('pool', lambda g, k_=k_: g.indirect_dma_start(out=xin[:, :], out_offset=bass.IndirectOffsetOnAxis(ap=dst_i[:, k_:k_ + 1], axis=0),
                                                                           in_=h2b[:], in_offset=None, bounds_check=bc_reg, oob_is_err=False),
                              reads=['dsti', 'h2b', 'xin'])
                    T.maybe_sync()
                T.sync_all()

        if on('experts'):
            with ExitStack() as s2:
                NB = CAP // 128
                w1s = [sb(s2, "E_w1_%d" % k, [128, KC, DE], BF16) for k in range(2)]
                w3s = [sb(s2, "E_w3_%d" % k, [128, KC, DE], BF16) for k in range(2)]
                w2s = [sb(s2, "E_w2_%d" % k, [128, 4, D], BF16) for k in range(2)]
                xb_ = [sb(s2, "E_xb%d" % k, [128, D], BF16) for k in range(2)]
                xT = sb(s2, "E_xT", [128, KC, CAP], BF16)
                sl = sb(s2, "E_sl", [128, 512], F32)
                hT = sb(s2, "E_hT", [128, 4, CAP], BF16)
                ost = [sb(s2, "E_ost%d" % k, [128, D], BF16) for k in range(2)]
                ptb = ps(s2, "E_ptb", [128, KC, 128], BF16)
                p1 = ps(s2, "E_p1", [128, 512], F32)
                p3 = ps(s2, "E_p3", [128, 512], F32)
                po = [ps(s2, "E_po%d" % k, [128, 512], F32) for k in range(2)]

                def load_w(e_):
                    k = e_ % 2
                    for kc in range(0, KC, 8):
                        T.dma('pool', lambda g, kc=kc: g.dma_start(out=w1s[k][:, kc:kc + 8, :], in_=w1[e_, kc * 128:(kc + 8) * 128, :].rearrange("(c p) n -> p c n", p=128)), writes=['E_w1_%d' % k])
                        T.dma('pool', lambda g, kc=kc: g.dma_start(out=w3s[k][:, kc:kc + 8, :], in_=w3[e_, kc * 128:(kc + 8) * 128, :].rearrange("(c p) n -> p c n", p=128)), writes=['E_w3_%d' % k])
                    for kc in range(0, 4, 2):
                        T.dma('pool', lambda g, kc=kc: g.dma_start(out=w2s[k][:, kc:kc + 2, :], in_=w2[e_, kc * 128:(kc + 2) * 128, :].rearrange("(c p) n -> p c n", p=128)), writes=['E_w2_%d' % k])

                load_w(0)
                for ei in range(NE):
                    if ei + 1 < NE:
                        load_w(ei + 1)
                    wk = ei % 2
                    W1, W3, W2 = w1s[wk], w3s[wk], w2s[wk]
                    n1, n3, n2 = 'E_w1_%d' % wk, 'E_w3_%d' % wk, 'E_w2_%d' % wk
                    xrows = xin[bass.ts(ei, CAP), :]
                    orows = eout[bass.ts(ei, CAP), :]
                    for b in range(NB):
                        k = b % 2
                        T.dma('sp', lambda e, b=b, k=k: e.dma_start(out=xb_[k][:], in_=xrows[b * 128:(b + 1) * 128, :]), writes=['E_xb%d' % k])
                        for c in range(KC):
                            T.op('pe', lambda e, c=c, k=k: e.transpose(out=ptb[:, c, :], in_=xb_[k][:, c * 128:(c + 1) * 128], identity=ident_b[:]),
                                 reads=['E_xb%d' % k, 'ident_b'], writes=['E_ptb%d' % (c // 8)])
                        T.op('act', lambda e, b=b: e.copy(out=xT[:, 0:8, b * 128:(b + 1) * 128], in_=ptb[:, 0:8, :]), reads=['E_ptb0'], writes=['E_xT%d' % b])
                        T.op('dve', lambda e, b=b: e.tensor_copy(out=xT[:, 8:16, b * 128:(b + 1) * 128], in_=ptb[:, 8:16, :]), reads=['E_ptb1'], writes=['E_xTb%d' % b])
                    xres = ['E_xT%d' % b for b in range(NB)] + ['E_xTb%d' % b for b in range(NB)]
                    for n0 in range(0, CAP, 512):
                        nn = min(512, CAP - n0)
                        for fb in range(4):
                            for c in range(KC):
                                T.op('pe', lambda e, c=c, fb=fb: e.matmul(p1[:, 0:nn], lhsT=W1[:, c, fb * 128:(fb + 1) * 128], rhs=xT[:, c, n0:n0 + nn], start=(c == 0), stop=(c == KC - 1)),
                                     reads=xres + [n1], writes=['E_p1'])
                            for c in range(KC):
                                T.op('pe', lambda e, c=c, fb=fb: e.matmul(p3[:, 0:nn], lhsT=W3[:, c, fb * 128:(fb + 1) * 128], rhs=xT[:, c, n0:n0 + nn], start=(c == 0), stop=(c == KC - 1)),
                                     reads=xres + [n3], writes=['E_p3'])
                            T.op('act', lambda e: e.activation(out=sl[:, 0:nn], in_=p1[:, 0:nn], func=AF.Silu), reads=['E_p1'], writes=['E_sl'])
                            T.op('dve', lambda e, fb=fb: e.tensor_tensor(out=hT[:, fb, n0:n0 + nn], in0=p3[:, 0:nn], in1=sl[:, 0:nn], op=ALU.mult), reads=['E_p3', 'E_sl'], writes=['E_hT'])
                    for b in range(NB):
                        k = b % 2
                        for j in range(4):
                            pk = po[j % 2]
                            for fb in range(4):
                                T.op('pe', lambda e, fb=fb, b=b, j=j: e.matmul(pk[:], lhsT=hT[:, fb, b * 128:(b + 1) * 128], rhs=W2[:, fb, j * 512:(j + 1) * 512], start=(fb == 0), stop=(fb == 3)),
                                     reads=['E_hT', n2], writes=['E_po%d' % (j % 2)])
                            if j % 2 == 0:
                                T.op('act', lambda e, j=j, k=k: e.copy(out=ost[k][:, j * 512:(j + 1) * 512], in_=pk[:]), reads=['E_po0'], writes=['E_ost%d' % k])
                            else:
                                T.op('dve', lambda e, j=j, k=k: e.tensor_copy(out=ost[k][:, j * 512:(j + 1) * 512], in_=pk[:]), reads=['E_po1'], writes=['E_ost%d' % k])
                        T.dma('sp', lambda e, b=b, k=k: e.dma_start(out=orows[b * 128:(b + 1) * 128, :], in_=ost[k][:]), reads=['E_ost%d' % k])
                    T.maybe_sync()
                T.sync_all()

        if on('combine'):
            with ExitStack() as s2:
                g0 = sb(s2, "C_g0", [128, D], BF16)
                g1 = sb(s2, "C_g1", [128, D], BF16)
                xm = sb(s2, "C_xm", [128, D], F32)
                acc = sb(s2, "C_acc", [128, D], F32)
                di = sb(s2, "C_di", [128, 2], I32)
                wv = sb(s2, "C_wv", [128, 2], F32)
                T.op('pool', lambda g: g.memset(g0[:], 0.0), writes=['C_g0'])
                T.op('pool', lambda g: g.memset(g1[:], 0.0), writes=['C_g1'])
                T.sync_all()
                for i in range(NT_OWN):
                    rows = bass.ts(i, 128)
                    T.dma('sp', lambda e: e.dma_start(out=di[:], in_=dest_d[rows, :]), writes=['C_di'])
                    T.dma('sp', lambda e: e.dma_start(out=wv[:], in_=wts_d[rows, :]), writes=['C_wv'])
                    T.dma('sp', lambda e: e.dma_start(out=xm[:], in_=x_mid[rows, :]), writes=['C_xm'])
                    for k_, gk in enumerate((g0, g1)):
                        T.dma('pool', lambda g, k_=k_, gk=gk: g.indirect_dma_start(out=gk[:], out_offset=None, in_=eout[:, :],
                                                                                  in_offset=bass.IndirectOffsetOnAxis(ap=di[:, k_:k_ + 1], axis=0),
                                                                                  bounds_check=bc_reg, oob_is_err=False), reads=['C_di'], writes=['C_g%d' % k_])
                    T.op('dve', lambda e: e.scalar_tensor_tensor(out=acc[:], in0=g0[:], scalar=wv[:, 0:1], in1=xm[:], op0=ALU.mult, op1=ALU.add), reads=['C_g0', 'C_wv', 'C_xm'], writes=['C_acc'])
                    T.op('dve', lambda e: e.scalar_tensor_tensor(out=acc[:], in0=g1[:], scalar=wv[:, 1:2], in1=acc[:], op0=ALU.mult, op1=ALU.add), reads=['C_g1', 'C_wv', 'C_acc'], writes=['C_acc'])
                    T.dma('sp', lambda e: e.dma_start(out=y[rows, :], in_=acc[:]), reads=['C_acc'])
                    T.maybe_sync()
                T.sync_all()
        T.sync_all()
    return nc, in_names


def _tabA(pos):
    j = np.arange(32, dtype=np.float32)
    inv = np.power(np.float32(10000.0), -j / np.float32(32)).astype(np.float32)
    r = (pos // 64).astype(np.float32)[:, None] * inv[None, :]
    c = (pos % 64).astype(np.float32)[:, None] * inv[None, :]
    return np.concatenate([np.cos(r), np.cos(c), np.sin(r), np.sin(c)], axis=1).astype(np.float32)


def _tabB(pos):
    j = np.arange(16, dtype=np.float32)
    inv = np.power(np.float32(500000.0), -j / np.float32(16)).astype(np.float32)
    a = pos.astype(np.float32)[:, None] * inv[None, :]
    return np.concatenate([np.cos(a), np.sin(a)], axis=1).astype(np.float32)


def make_in_maps(inputs, OWN):
    xp, xs = np.asarray(inputs['x_prompt']), np.asarray(inputs['x_sample'])
    shared = {
        'g_mix': np.ascontiguousarray(inputs['g_mix'][0][None, :]), 'g_ffn': np.ascontiguousarray(inputs['g_ffn'][0][None, :]),
        'g_qa': np.ascontiguousarray(inputs['g_qa'][0][None, :]), 'g_ka': np.ascontiguousarray(inputs['g_ka'][0][None, :]),
        'g_qb': np.ascontiguousarray(inputs['g_qb'][0][None, :]), 'g_kb': np.ascontiguousarray(inputs['g_kb'][0][None, :]),
        'w_in': np.ascontiguousarray(inputs['w_in'][0]), 'w_oa': np.ascontiguousarray(inputs['w_oa'][0]),
        'w_ob': np.ascontiguousarray(inputs['w_ob'][0]), 'w_out': np.ascontiguousarray(inputs['w_out'][0]),
        'w_r': np.ascontiguousarray(np.concatenate([inputs['w_rg'][0], inputs['w_re'][0]], axis=1)),
        'b_r': np.ascontiguousarray(np.concatenate([inputs['b_rg'][0], inputs['b_re'][0]])[None, :]),
        'w1': np.ascontiguousarray(inputs['w1'][0]), 'w3': np.ascontiguousarray(inputs['w3'][0]), 'w2': np.ascontiguousarray(inputs['w2'][0]),
    }
    maps = []
    for c in range(8):
        chunks = [(xp[c // 2], (c % 2) * OWN), (xs[c // 4], (c % 4) * OWN)]
        xo, xr, xh, tAo, tBo, tAr, tBh, vh = [], [], [], [], [], [], [], []
        for (seq, p0) in chunks:
            S = seq.shape[0]
            own = np.arange(p0, p0 + OWN)
            rest = np.concatenate([np.arange(0, p0), np.arange(p0 + OWN, S)])
            xo.append(seq[own]); tAo.append(_tabA(own)); tBo.append(_tabB(own))
            xr.append(seq[rest]); tAr.append(_tabA(rest))
            for hp in (np.arange(p0 - HALO, p0), np.arange(p0 + OWN, p0 + OWN + HALO)):
                ok = (hp >= 0) & (hp < S)
                blk = np.zeros((HALO, D), np.float32)
                blk[ok] = seq[hp[ok]]
                xh.append(blk); tBh.append(_tabB(np.clip(hp, 0, S - 1))); vh.append(ok.astype(np.float32)[:, None])
        m = dict(shared)
        m.update(xo=np.concatenate(xo), xr=np.concatenate(xr), xh=np.concatenate(xh), tA_o=np.concatenate(tAo), tB_o=np.concatenate(tBo),
                 tA_r=np.concatenate(tAr), tB_h=np.concatenate(tBh),
                 vh=np.concatenate(vh).reshape(-1, TB, 128).transpose(0, 2, 1).reshape(-1, TB))
        maps.append({k: np.ascontiguousarray(v) for k, v in m.items()})
    return maps


def run(inputs, OWN, CAP, stages=None, dbg=(), cores=8):
    nc, in_names = build(OWN, CAP, stages=stages, dbg=dbg)
    maps = [{k: m[k] for k in in_names} for m in make_in_maps(inputs, OWN)[:cores]]
    res = run_bass_kernel_spmd(nc, maps, core_ids=list(range(cores)))
    return res.results


def kernel(**inputs):
    OWN = 4096
    res = run(inputs, OWN, 640)
    yp = np.empty((4, 8192, D), np.float32)
    ys = np.empty((2, 16384, D), np.float32)
    for c in range(8):
        yc = res[c]['y']
        yp[c // 2, (c % 2) * OWN:(c % 2 + 1) * OWN] = yc[:OWN]
        ys[c // 4, (c % 4) * OWN:(c % 4 + 1) * OWN] = yc[OWN:]
    return (yp, ys)
```
